# Optimizing a Trainium2 kernel written in Bass

```python
import jax, jax.numpy as jnp
from jax import lax
import numpy as np

D_MODEL = 1024
BATCH = 8
SEQ = 4096
DEPTH = 2

GRID_W = 64
CTX_LEN = 256
HEAD_DIM = 64
SWA_HEADS = 6
SWA_KV_HEADS = 2
WINDOW = 128
GLB_HEADS = 6
GLB_KV_HEADS = 2
MLA_HEADS = 4
MLA_NOPE_DIM = 64
MLA_ROPE_DIM = 32
MLA_V_DIM = 64
MLA_Q_RANK = 256
MLA_KV_RANK = 128
MIX_WIDTH = (SWA_HEADS + GLB_HEADS) * HEAD_DIM + MLA_HEADS * MLA_V_DIM
BLOCK = 128
ROPE_THETA = 10000.0
N_EXPERTS = 16
N_GROUPS = 4
EXPERTS_PER_GROUP = N_EXPERTS // N_GROUPS
TOPK_GROUPS = 1
GROUP_SCORE_TOPK = 2
TOP_K = 2
D_EXPERT = 512
D_SHARED = 512
MOE_BLOCK = 128
EPS = 1e-6
NEG_INF = -1e30

IN_SPLIT_SIZES = (SWA_HEADS * HEAD_DIM, SWA_KV_HEADS * HEAD_DIM, SWA_KV_HEADS * HEAD_DIM,
                  GLB_HEADS * HEAD_DIM, GLB_KV_HEADS * HEAD_DIM, GLB_KV_HEADS * HEAD_DIM,
                  MLA_Q_RANK, MLA_KV_RANK, MLA_ROPE_DIM)
IN_WIDTH = sum(IN_SPLIT_SIZES)
IN_SPLIT_POINTS = tuple(int(p) for p in np.cumsum(IN_SPLIT_SIZES)[:-1])

kernel_name = "hymba_hybrid_dit_mla_swa_axial_moe"


def rmsnorm(x, g):
    xf = x.astype(jnp.float32)
    y = xf * lax.rsqrt(jnp.mean(xf * xf, axis=-1, keepdims=True) + EPS)
    return (y * g.astype(jnp.float32)).astype(x.dtype)


def modulate(h, shift, scale):
    return h * (1 + scale) + shift


def swiglu(h, w_gate, w_up, w_down):
    return (jax.nn.silu(h @ w_gate) * (h @ w_up)) @ w_down


def axial_rope(x, rows, cols):
    d = x.shape[-1]
    half = d // 2
    nf = half // 2
    inv = ROPE_THETA ** (-jnp.arange(nf, dtype=jnp.float32) / nf)
    bshape = (1, x.shape[1]) + (1,) * (x.ndim - 3) + (nf,)

    def rotate(xa, pos):
        ang = pos.astype(jnp.float32)[:, None] * inv
        cos = jnp.cos(ang).reshape(bshape)
        sin = jnp.sin(ang).reshape(bshape)
        x1 = xa[..., :nf].astype(jnp.float32)
        x2 = xa[..., nf:].astype(jnp.float32)
        return jnp.concatenate([x1 * cos - x2 * sin, x2 * cos + x1 * sin], axis=-1)

    return jnp.concatenate([rotate(x[..., :half], rows), rotate(x[..., half:], cols)], axis=-1).astype(x.dtype)


def attend(q, k, v, scale, mask=None, sink=None):
    s = jnp.einsum("...qhgd,...khd->...hgqk", q, k).astype(jnp.float32) * scale
    if mask is not None:
        s = jnp.where(mask, s, NEG_INF)
    if sink is not None:
        sink_logit = sink.astype(jnp.float32).reshape(s.shape[-4], s.shape[-3], 1, 1)
        sink_logit = jnp.broadcast_to(sink_logit, s.shape[:-1] + (1,))
        p = jax.nn.softmax(jnp.concatenate([s, sink_logit], axis=-1), axis=-1)[..., :-1]
    else:
        p = jax.nn.softmax(s, axis=-1)
    return jnp.einsum("...hgqk,...khd->...qhgd", p.astype(v.dtype), v)


def window_attention(q, k, v, k_ctx, v_ctx, sink):
    bsz, seq = q.shape[:2]
    nb = seq // BLOCK
    n_ctx = k_ctx.shape[1]
    halo = ((0, 0), (BLOCK, BLOCK), (0, 0), (0, 0))
    k_pad = jnp.pad(k, halo)
    v_pad = jnp.pad(v, halo)
    q_blocks = jnp.moveaxis(q.reshape((bsz, nb, BLOCK) + q.shape[2:]), 1, 0)
    offs_q = jnp.arange(BLOCK)
    offs_k = jnp.arange(3 * BLOCK) - BLOCK
    ctx_ok = jnp.ones((BLOCK, n_ctx), dtype=bool)
    scale = HEAD_DIM ** -0.5

    def one_block(args):
        q_blk, i = args
        start = i * BLOCK
        k_loc = lax.dynamic_slice_in_dim(k_pad, start, 3 * BLOCK, axis=1)
        v_loc = lax.dynamic_slice_in_dim(v_pad, start, 3 * BLOCK, axis=1)
        q_pos = start + offs_q
        k_pos = start + offs_k
        loc_ok = ((jnp.abs(q_pos[:, None] - k_pos[None, :]) <= WINDOW)
                  & (k_pos >= 0)[None, :] & (k_pos < seq)[None, :])
        mask = jnp.concatenate([ctx_ok, loc_ok], axis=1)
        return attend(q_blk, jnp.concatenate([k_ctx, k_loc], axis=1),
                      jnp.concatenate([v_ctx, v_loc], axis=1), scale, mask, sink)

    out = lax.map(one_block, (q_blocks, jnp.arange(nb)))
    return jnp.moveaxis(out, 0, 1).reshape(bsz, seq, -1)


def block_sweep_attention(q, k, v, scale):
    bsz, seq = q.shape[:2]
    nb = seq // BLOCK
    q_blocks = jnp.moveaxis(q.reshape((bsz, nb, BLOCK) + q.shape[2:]), 1, 0)
    out = lax.map(lambda q_blk: attend(q_blk, k, v, scale), q_blocks)
    return jnp.moveaxis(out, 0, 1).reshape(bsz, seq, -1)


def mixer_inputs(h, w_in, glb_q_gain, glb_k_gain, mla_q_gain, mla_w_uq, mla_kv_gain, mla_w_ukv):
    bsz, n = h.shape[:2]
    sq, sk, sv, gq, gk, gv, mq_lat, mkv_lat, mk_rope = jnp.split(h @ w_in, IN_SPLIT_POINTS, axis=-1)
    swa = (sq.reshape(bsz, n, SWA_KV_HEADS, SWA_HEADS // SWA_KV_HEADS, HEAD_DIM),
           sk.reshape(bsz, n, SWA_KV_HEADS, HEAD_DIM),
           sv.reshape(bsz, n, SWA_KV_HEADS, HEAD_DIM))
    glb = (rmsnorm(gq.reshape(bsz, n, GLB_KV_HEADS, GLB_HEADS // GLB_KV_HEADS, HEAD_DIM), glb_q_gain),
           rmsnorm(gk.reshape(bsz, n, GLB_KV_HEADS, HEAD_DIM), glb_k_gain),
           gv.reshape(bsz, n, GLB_KV_HEADS, HEAD_DIM))
    mq = (rmsnorm(mq_lat, mla_q_gain) @ mla_w_uq).reshape(bsz, n, MLA_HEADS, 1, MLA_NOPE_DIM + MLA_ROPE_DIM)
    mkv = (rmsnorm(mkv_lat, mla_kv_gain) @ mla_w_ukv).reshape(bsz, n, MLA_HEADS, MLA_NOPE_DIM + MLA_V_DIM)
    mla = (mq[..., :MLA_NOPE_DIM], mq[..., MLA_NOPE_DIM:],
           mkv[..., :MLA_NOPE_DIM], mk_rope.reshape(bsz, n, 1, MLA_ROPE_DIM), mkv[..., MLA_NOPE_DIM:])
    return swa, glb, mla


def mla_queries(q_nope, q_rope):
    return jnp.concatenate([q_nope, q_rope], axis=-1)


def mla_keys(k_nope, k_rope):
    k_rope = jnp.broadcast_to(k_rope, k_nope.shape[:-1] + (MLA_ROPE_DIM,))
    return jnp.concatenate([k_nope, k_rope], axis=-1)


def routed_experts(h, idx, gate_w, w_gate, w_up, w_down):
    n_tok, d = h.shape
    n_assign = n_tok * TOP_K
    flat_e = idx.reshape(-1)
    order = jnp.argsort(flat_e)
    sorted_e = flat_e[order]
    counts = jnp.bincount(flat_e, length=N_EXPERTS)
    padded = (counts + MOE_BLOCK - 1) // MOE_BLOCK * MOE_BLOCK
    pad_end = jnp.cumsum(padded)
    pad_start = pad_end - padded
    start = jnp.cumsum(counts) - counts
    dest = pad_start[sorted_e] + jnp.arange(n_assign) - start[sorted_e]
    n_blocks = -(-n_assign // MOE_BLOCK) + N_EXPERTS
    src_tok = order // TOP_K
    slot_tok = jnp.full((n_blocks * MOE_BLOCK,), n_tok, jnp.int32).at[dest].set(src_tok)
    h_slots = jnp.concatenate([h, jnp.zeros((1, d), h.dtype)], axis=0)[slot_tok]
    block_e = jnp.minimum(jnp.searchsorted(pad_end, jnp.arange(n_blocks) * MOE_BLOCK, side="right"),
                          N_EXPERTS - 1)

    def expert_block(args):
        hb, e = args
        return swiglu(hb, w_gate[e], w_up[e], w_down[e])

    y = lax.map(expert_block, (h_slots.reshape(n_blocks, MOE_BLOCK, d), block_e)).reshape(-1, d)
    contrib = y[dest] * gate_w.reshape(-1)[order][:, None].astype(y.dtype)
    return jax.ops.segment_sum(contrib, src_tok, num_segments=n_tok)


def moe_ffn(h, router_w, router_bias, w_gate, w_up, w_down, sw_gate, sw_up, sw_down):
    n_tok = h.shape[0]
    scores = jax.nn.sigmoid((h @ router_w).astype(jnp.float32))
    biased = scores + router_bias.astype(jnp.float32)
    grp_score = lax.top_k(biased.reshape(n_tok, N_GROUPS, EXPERTS_PER_GROUP), GROUP_SCORE_TOPK)[0].sum(-1)
    _, grp_idx = lax.top_k(grp_score, TOPK_GROUPS)
    grp_sel = jnp.any(grp_idx[..., None] == jnp.arange(N_GROUPS), axis=-2)
    expert_ok = jnp.repeat(grp_sel, EXPERTS_PER_GROUP, axis=-1)
    _, idx = lax.top_k(jnp.where(expert_ok, biased, NEG_INF), TOP_K)
    w = jnp.take_along_axis(scores, idx, axis=-1)
    w = w / jnp.sum(w, axis=-1, keepdims=True)
    return routed_experts(h, idx, w, w_gate, w_up, w_down) + swiglu(h, sw_gate, sw_up, sw_down)


def setup_inputs(seed: int = 0) -> dict:
    key = jax.random.key(seed)
    ks = jax.random.split(key, 26)
    L, D, E = DEPTH, D_MODEL, N_EXPERTS

    def nrm(k, shape, s):
        return jax.random.normal(k, shape, jnp.float32) * s

    return {
        "x": nrm(ks[0], (BATCH, SEQ, D), 1.0),
        "c": nrm(ks[1], (BATCH, D), 1.0),
        "ctx": nrm(ks[2], (BATCH, CTX_LEN, D), 1.0),
        "c_ctx": nrm(ks[3], (D,), 1.0),
        "w_mod": nrm(ks[4], (L, D, 6 * D), 0.5 * D ** -0.5),
        "b_mod": nrm(ks[5], (L, 6 * D), 0.02),
        "norm_mix_g": 1.0 + nrm(ks[6], (L, D), 0.1),
        "norm_ffn_g": 1.0 + nrm(ks[7], (L, D), 0.1),
        "w_in": nrm(ks[8], (L, D, IN_WIDTH), D ** -0.5),
        "w_out": nrm(ks[9], (L, MIX_WIDTH, D), MIX_WIDTH ** -0.5),
        "swa_sink": nrm(ks[10], (L, SWA_HEADS), 0.5),
        "glb_q_gain": 1.0 + nrm(ks[11], (L, HEAD_DIM), 0.1),
        "glb_k_gain": 1.0 + nrm(ks[12], (L, HEAD_DIM), 0.1),
        "mla_q_gain": 1.0 + nrm(ks[13], (L, MLA_Q_RANK), 0.1),
        "mla_w_uq": nrm(ks[14], (L, MLA_Q_RANK, MLA_HEADS * (MLA_NOPE_DIM + MLA_ROPE_DIM)), MLA_Q_RANK ** -0.5),
        "mla_kv_gain": 1.0 + nrm(ks[15], (L, MLA_KV_RANK), 0.1),
        "mla_w_ukv": nrm(ks[16], (L, MLA_KV_RANK, MLA_HEADS * (MLA_NOPE_DIM + MLA_V_DIM)), MLA_KV_RANK ** -0.5),
        "router_w": nrm(ks[17], (D, E), D ** -0.5),
        "router_bias": nrm(ks[18], (E,), 0.01),
        "exp_w_gate": nrm(ks[19], (L, E, D, D_EXPERT), D ** -0.5),
        "exp_w_up": nrm(ks[20], (L, E, D, D_EXPERT), D ** -0.5),
        "exp_w_down": nrm(ks[21], (L, E, D_EXPERT, D), D_EXPERT ** -0.5),
        "shr_w_gate": nrm(ks[22], (L, D, D_SHARED), D ** -0.5),
        "shr_w_up": nrm(ks[23], (L, D, D_SHARED), D ** -0.5),
        "shr_w_down": nrm(ks[24], (L, D_SHARED, D), D_SHARED ** -0.5),
        "final_norm_g": 1.0 + nrm(ks[25], (D,), 0.1),
    }


def reference(x, c, ctx, c_ctx, w_mod, b_mod, norm_mix_g, norm_ffn_g, w_in, w_out, swa_sink,
              glb_q_gain, glb_k_gain, mla_q_gain, mla_w_uq, mla_kv_gain, mla_w_ukv,
              router_w, router_bias, exp_w_gate, exp_w_up, exp_w_down,
              shr_w_gate, shr_w_up, shr_w_down, final_norm_g):
    bsz, seq, d = x.shape
    n_ctx = ctx.shape[1]
    rows_n = seq // GRID_W
    rows = jnp.repeat(jnp.arange(rows_n), GRID_W)
    cols = jnp.tile(jnp.arange(GRID_W), rows_n)
    hd_scale = HEAD_DIM ** -0.5
    mla_scale = (MLA_NOPE_DIM + MLA_ROPE_DIM) ** -0.5

    for l in range(DEPTH):
        last = l == DEPTH - 1
        mod_x = jnp.split((jax.nn.silu(c) @ w_mod[l] + b_mod[l])[:, None, :], 6, axis=-1)
        mod_c = jnp.split(jax.nn.silu(c_ctx) @ w_mod[l] + b_mod[l], 6, axis=-1)
        mix_params = (w_in[l], glb_q_gain[l], glb_k_gain[l], mla_q_gain[l], mla_w_uq[l],
                      mla_kv_gain[l], mla_w_ukv[l])

        h = modulate(rmsnorm(x, norm_mix_g[l]), mod_x[0], mod_x[1])
        hc = modulate(rmsnorm(ctx, norm_mix_g[l]), mod_c[0], mod_c[1])
        (sq, sk, sv), (gq, gk, gv), (mqn, mqr, mkn, mkr, mv) = mixer_inputs(h, *mix_params)
        (csq, csk, csv), (cgq, cgk, cgv), (cmqn, cmqr, cmkn, cmkr, cmv) = mixer_inputs(hc, *mix_params)
        cmk = mla_keys(cmkn, cmkr)

        y_swa = window_attention(axial_rope(sq, rows, cols), axial_rope(sk, rows, cols), sv,
                                 csk, csv, swa_sink[l])
        y_glb = block_sweep_attention(axial_rope(gq, rows, cols),
                                      jnp.concatenate([cgk, axial_rope(gk, rows, cols)], axis=1),
                                      jnp.concatenate([cgv, gv], axis=1), hd_scale)
        y_mla = block_sweep_attention(mla_queries(mqn, axial_rope(mqr, rows, cols)),
                                      jnp.concatenate([cmk, mla_keys(mkn, axial_rope(mkr, rows, cols))], axis=1),
                                      jnp.concatenate([cmv, mv], axis=1), mla_scale)
        x = x + mod_x[2] * (jnp.concatenate([y_swa, y_glb, y_mla], axis=-1) @ w_out[l])

        h = modulate(rmsnorm(x, norm_ffn_g[l]), mod_x[3], mod_x[4]).reshape(-1, d)
        moe_params = (router_w, router_bias, exp_w_gate[l], exp_w_up[l], exp_w_down[l],
                      shr_w_gate[l], shr_w_up[l], shr_w_down[l])
        if last:
            f = moe_ffn(h, *moe_params)
        else:
            yc = jnp.concatenate([
                attend(csq, csk, csv, hd_scale, sink=swa_sink[l]).reshape(bsz, n_ctx, -1),
                attend(cgq, cgk, cgv, hd_scale).reshape(bsz, n_ctx, -1),
                attend(mla_queries(cmqn, cmqr), cmk, cmv, mla_scale).reshape(bsz, n_ctx, -1)], axis=-1)
            ctx = ctx + mod_c[2] * (yc @ w_out[l])
            hc = modulate(rmsnorm(ctx, norm_ffn_g[l]), mod_c[3], mod_c[4]).reshape(-1, d)
            f_all = moe_ffn(jnp.concatenate([h, hc], axis=0), *moe_params)
            f = f_all[: bsz * seq]
            ctx = ctx + mod_c[5] * f_all[bsz * seq:].reshape(ctx.shape)
        x = x + mod_x[5] * f.reshape(x.shape)

    return rmsnorm(x, final_norm_g)
```

```python
import numpy as np
import concourse.bass as bass
import concourse.mybir as mybir
from concourse.bass_utils import run_bass_kernel_spmd

F32 = mybir.dt.float32
BF16 = mybir.dt.bfloat16
I32 = mybir.dt.int32
U32 = mybir.dt.uint32
ALU = mybir.AluOpType
AF = mybir.ActivationFunctionType

ENGS = ("pe", "dve", "act", "pool", "sp")
NDMASEM = 24


class Op:
    __slots__ = ("eng", "fn", "deps", "needs_inc", "val", "dma", "dsem", "dval", "idx", "epoch")

    def __init__(self, eng, fn, dma=False):
        self.eng = eng
        self.fn = fn
        self.deps = []
        self.needs_inc = False
        self.val = 0
        self.dma = dma
        self.dsem = None
        self.dval = 0
        self.idx = 0
        self.epoch = 0


class Prog:
    def __init__(self, nc):
        self.nc = nc
        self.ops = {e: [] for e in ENGS}
        self.last_w = {}
        self.readers = {}
        self.ndma = 0
        self.dma_prev = [None] * NDMASEM
        self.all_ops = []
        self.epoch = 0

    def _add(self, eng, fn, reads, writes, dma=False):
        op = Op(eng, fn, dma)
        op.idx = len(self.ops[eng])
        op.epoch = self.epoch
        deps = []
        for k in reads:
            w = self.last_w.get(k)
            if w is not None:
                deps.append(w)
            if isinstance(k, tuple) and k and k[0] == "ps":
                for r in self.readers.get(k, ()):
                    if r.eng != eng:
                        deps.append(r)
        for k in writes:
            w = self.last_w.get(k)
            if w is not None:
                deps.append(w)
            for r in self.readers.get(k, ()):
                if r.eng != eng or r.dma or dma:
                    deps.append(r)
        if dma:
            s = self.ndma % NDMASEM
            self.ndma += 1
            prev = self.dma_prev[s]
            op.dsem = s
            op.dval = (prev.dval if prev is not None else 0) + 16
            if prev is not None:
                deps.append(prev)
            self.dma_prev[s] = op
        seen = set()
        for d in deps:
            if d is op or id(d) in seen:
                continue
            seen.add(id(d))
            if (not d.dma) and d.eng == eng and eng == "pe" and not dma:
                continue
            op.deps.append(d)
            if not d.dma:
                d.needs_inc = True
        for k in reads:
            self.readers.setdefault(k, []).append(op)
        for k in writes:
            self.last_w[k] = op
            self.readers[k] = []
        self.ops[eng].append(op)
        self.all_ops.append(op)
        return op

    def pe(self, fn, reads=(), writes=()):
        return self._add("pe", fn, reads, writes)

    def dve(self, fn, reads=(), writes=()):
        return self._add("dve", fn, reads, writes)

    def act(self, fn, reads=(), writes=()):
        return self._add("act", fn, reads, writes)

    def pool(self, fn, reads=(), writes=()):
        return self._add("pool", fn, reads, writes)

    def dma(self, fn, reads=(), writes=(), q="sp"):
        return self._add(q, fn, reads, writes, dma=True)

    def barrier(self, bump=True):
        key = ("__barrier__", len(self.all_ops))
        lasts = []
        for e in ENGS:
            for op in reversed(self.ops[e]):
                if (not op.dma) and op.fn is not None:
                    lasts.append(op)
                    break
        for d in self.dma_prev:
            if d is not None:
                lasts.append(d)
        self._barrier_deps = lasts
        self.pending_barrier = {e: list(lasts) for e in ENGS}
        for e in ENGS:
            op = Op(e, None, False)
            op.idx = len(self.ops[e])
            for d in lasts:
                if d.eng == e and not d.dma:
                    continue
                op.deps.append(d)
                if not d.dma:
                    d.needs_inc = True
            self.ops[e].append(op)
        self.last_w = {}
        self.readers = {}
        if bump:
            self.epoch += 1

    def emit(self, final_waits=True):
        nc = self.nc
        used = set()
        for e in ENGS:
            c = {}
            for op in self.ops[e]:
                if op.dma:
                    continue
                if op.needs_inc:
                    c[op.epoch] = c.get(op.epoch, 0) + 1
                    op.val = c[op.epoch]
                    used.add((e, op.epoch))
        import contextlib

        with contextlib.ExitStack() as st:
            esem = {(e, ep): st.enter_context(nc.semaphore("s_%s_%d" % (e, ep))) for (e, ep) in sorted(used)}
            dsem = [st.enter_context(nc.semaphore("d_%d" % i)) for i in range(NDMASEM)]
            block = st.enter_context(nc.Block())

            def run(e, eng):
                waited = {}
                for op in self.ops[e]:
                    for d in op.deps:
                        if d.dma:
                            key, val, sem = ("d", d.dsem), d.dval, dsem[d.dsem]
                        else:
                            key, val, sem = (d.eng, d.epoch), d.val, esem[(d.eng, d.epoch)]
                        if waited.get(key, 0) >= val:
                            continue
                        eng.wait_ge(sem, val)
                        waited[key] = val
                    if op.fn is None:
                        continue
                    ins = op.fn(eng)
                    if op.dma:
                        ins.then_inc(dsem[op.dsem], 16)
                    elif op.needs_inc:
                        ins.then_inc(esem[(e, op.epoch)], 1)
                if final_waits:
                    for d in self.dma_prev:
                        if d is not None and d.eng == e:
                            if waited.get(("d", d.dsem), 0) < d.dval:
                                eng.wait_ge(dsem[d.dsem], d.dval)

            @block.tensor
            def _(eng):
                run("pe", eng)

            @block.vector
            def _(eng):
                run("dve", eng)

            @block.scalar
            def _(eng):
                run("act", eng)

            @block.gpsimd
            def _(eng):
                run("pool", eng)

            @block.sync
            def _(eng):
                run("sp", eng)


AX = mybir.AxisListType
D = 1024
SEQ = 4096
NCTX = 256
NT = SEQ + NCTX
NL = 2
CH = [(i * 512, 512) for i in range(8)] + [(4096, 256)]
NKB = NT // 128
EPS = 1e-6
ARENA_WORDS = 48500
DEBUG = False


class Arena:
    def __init__(self, nc, words):
        self.t = nc.alloc_sbuf_tensor("arena", [128, words], F32).ap()
        self.words = words
        self.off = 0

    def reset(self, off=0):
        self.off = off

    def tile(self, dtype, shape):
        n = int(np.prod(shape))
        nbytes = n * (2 if dtype == BF16 else 4)
        w = (nbytes + 3) // 4
        w = (w + 7) // 8 * 8
        o = self.off
        self.off += w
        assert self.off <= self.words, "arena overflow %d" % self.off
        v = self.t[:, o:o + w]
        if dtype != F32:
            v = v.bitcast(dtype)
        v = v[:, 0:n]
        if len(shape) == 2:
            v = v.rearrange("p (a b) -> p a b", a=shape[0])
        elif len(shape) == 3:
            v = v.rearrange("p (a b c) -> p a b c", a=shape[0], b=shape[1])
        return v


class K:
    def __init__(self, P):
        self.P = P

    def mm(self, out, lhsT, rhs, start=True, stop=True, reads=(), writes=()):
        return self.P.pe(lambda e: e.matmul(out, lhsT=lhsT, rhs=rhs, start=start, stop=stop), reads, writes)

    def transpose(self, out, in_, ident, reads=(), writes=()):
        return self.P.pe(lambda e: e.transpose(out, in_, ident), reads, writes)

    def act(self, out, in_, func, bias=None, scale=1.0, reads=(), writes=()):
        if bias is None:
            return self.P.act(lambda e: e.activation(out=out, in_=in_, func=func, scale=scale), reads, writes)
        return self.P.act(lambda e: e.activation(out=out, in_=in_, func=func, bias=bias, scale=scale), reads, writes)

    def _eng(self, eng):
        return {"dve": self.P.dve, "pool": self.P.pool}[eng]

    def tt(self, eng, out, in0, in1, op, reads=(), writes=()):
        return self._eng(eng)(lambda e: e.tensor_tensor(out=out, in0=in0, in1=in1, op=op), reads, writes)

    def ts(self, eng, out, in0, s1, op0, s2=None, op1=None, reads=(), writes=()):
        if op1 is None:
            return self._eng(eng)(lambda e: e.tensor_scalar(out=out, in0=in0, scalar1=s1, scalar2=None, op0=op0), reads, writes)
        return self._eng(eng)(lambda e: e.tensor_scalar(out=out, in0=in0, scalar1=s1, scalar2=s2, op0=op0, op1=op1), reads, writes)

    def stt(self, out, in0, scalar, in1, op0, op1, reads=(), writes=()):
        return self.P.dve(lambda e: e.scalar_tensor_tensor(out=out, in0=in0, scalar=scalar, in1=in1, op0=op0, op1=op1), reads, writes)

    def copy(self, eng, out, in_, reads=(), writes=()):
        return self._eng(eng)(lambda e: e.tensor_copy(out=out, in_=in_), reads, writes)

    def recip(self, out, in_, reads=(), writes=()):
        return self.P.dve(lambda e: e.reciprocal(out=out, in_=in_), reads, writes)

    def reduce(self, out, in_, op, axis, reads=(), writes=()):
        return self.P.dve(lambda e: e.tensor_reduce(out=out, in_=in_, axis=axis, op=op), reads, writes)

    def memset(self, eng, ap, val, writes=()):
        return self._eng(eng)(lambda e: e.memset(ap, val), (), writes)

    def dma(self, out, in_, reads=(), writes=(), q="sp"):
        return self.P.dma(lambda e: e.dma_start(out=out, in_=in_), reads, writes, q=q)


def build_program(dbg=False):
    nc = bass.Bass("TRN2", target_bir_lowering=False)
    P = Prog(nc)
    k = K(P)

    def din(name, shape, dt=F32):
        return nc.dram_tensor(name, list(shape), dt, kind="ExternalInput").ap()

    def dscr(name, shape, dt):
        if dbg:
            return nc.dram_tensor(name, list(shape), dt, kind="ExternalOutput").ap()
        return nc.dram_tensor(name, list(shape), dt).ap()

    xT = din("xT", [D, SEQ])
    ctxT = din("ctxT", [D, NCTX])
    cc = din("cc", [128, 16])
    w_mod = din("w_mod", [NL, D, 6 * D])
    bm = din("bm", [NL, 128, 48])
    g1 = din("g1", [NL, 128, 8])
    g2 = din("g2", [NL, 128, 8])
    gf = din("gf", [128, 8])
    w_in = din("w_in", [NL, D, 1696])
    w_in_sw = din("w_in_sw", [NL, D, 1696])
    w_out = din("w_out", [NL, D, D])
    sink = din("sink", [NL, 1, 6])
    gqk = din("gqk", [NL, 64, 4])
    gmq = din("gmq", [NL, 128, 2])
    gmkv = din("gmkv", [NL, 128, 1])
    w_uq = din("w_uq", [NL, 256, 384])
    w_uq_sw = din("w_uq_sw", [NL, 256, 384])
    w_ukv = din("w_ukv", [NL, 128, 512])
    rw = din("rw", [128, 8 * 16])
    rb = din("rb", [16])
    ewg = din("ewg", [NL, 16, D, 512])
    ewu = din("ewu", [NL, 16, D, 512])
    ewd = din("ewd", [NL, 16, 512, D])
    swg = din("swg", [NL, D, 512])
    swu = din("swu", [NL, D, 512])
    swd = din("swd", [NL, 512, D])
    tabs = din("tabs", [4, 128, NT])
    masks = din("masks", [128, 6 * 512])
    ident_d = din("ident", [128, 128])
    outT = nc.dram_tensor("outT", [D, SEQ], F32, kind="ExternalOutput").ap()

    HT = [dscr("HT%d" % l, [8, 128, NT], BF16) for l in range(NL)]
    HT2 = [dscr("HT2_%d" % l, [8, 128, NT], BF16) for l in range(NL)]
    QS = [dscr("QS%d" % l, [16, 96, NT], BF16) for l in range(NL)]
    YS = [dscr("YS%d" % l, [16, 65, NT], BF16) for l in range(NL)]
    X1 = [dscr("X1_%d" % l, [8, 128, NT], F32) for l in range(NL)]
    X2 = [dscr("X2_%d" % l, [8, 128, NT], F32) for l in range(NL)]

    def xsrc(l, c):
        t0, W = CH[c]
        if l == 0:
            if c < 8:
                return xT[:, t0:t0 + W].rearrange("(k p) t -> p k t", p=128)
            return ctxT.rearrange("(k p) t -> p k t", p=128)
        return X2[l - 1][:, :, t0:t0 + W].rearrange("k p t -> p k t")

    def sview(T, c):
        t0, W = CH[c]
        return T[:, :, t0:t0 + W].rearrange("k p t -> p k t")

    def sb(name, shape, dt=F32):
        return nc.alloc_sbuf_tensor(name, list(shape), dt).ap()

    ones_bf = sb("ones_bf", [128, 128], BF16)
    ones_f = sb("ones_f", [128, 128], F32)
    ident = sb("ident_sb", [128, 128], F32)
    epsb = sb("epsb", [128, 1], F32)
    cc_t = sb("cc_t", [128, 16], F32)
    sc_t = sb("sc_t", [128, 16], F32)
    modv = sb("modv", [128, 96], F32)
    bm_t = sb("bm_t", [128, 48], F32)
    g1_t = sb("g1_t", [128, 8], F32)
    g2_t = sb("g2_t", [128, 8], F32)
    gf_t = sb("gf_t", [128, 8], F32)
    A1 = sb("A1", [128, 16], F32)
    A2 = sb("A2", [128, 16], F32)
    gqk_t = sb("gqk_t", [64, 4], F32)
    gmq_t = sb("gmq_t", [128, 2], F32)
    gmkv_t = sb("gmkv_t", [128, 1], F32)
    sink_t = sb("sink_t", [1, 6], F32)
    rw_t = sb("rw_t", [128, 128], F32)
    rb_t = sb("rb_t", [128, 16], F32)
    Gs = sb("Gs", [128, NKB * 16], F32)
    rt = [sb("rt%d" % i, [128, 160], F32) for i in range(2)]

    psbig = nc.alloc_psum_tensor("psbig", [128, 4096], F32).ap()
    ps = [psbig[:, i * 512:(i + 1) * 512] for i in range(8)]
    PSK = [("ps", i) for i in range(8)]

    A = Arena(nc, ARENA_WORDS)

    modv3 = modv.rearrange("p (j c) -> p j c", c=2)
    A13 = A1.rearrange("p (j c) -> p j c", c=2)
    A23 = A2.rearrange("p (j c) -> p j c", c=2)
    sc3 = sc_t.rearrange("p (j c) -> p j c", c=2)
    rw3 = rw_t.rearrange("p (k e) -> p k e", e=16)
    Gs3 = Gs.rearrange("p (t e) -> p t e", e=16)

    k.memset("pool", ones_bf, 1.0, writes=["ones_bf"])
    k.memset("pool", ones_f, 1.0, writes=["ones_f"])
    k.memset("pool", epsb, EPS, writes=["epsb"])
    k.dma(ident, ident_d, writes=["ident"])
    k.dma(cc_t, cc, writes=["cc_t"])
    k.dma(gf_t, gf, writes=["gf_t"])
    k.dma(rw_t, rw, writes=["rw_t"])
    k.dma(rb_t, rb.partition_broadcast(128), writes=["rb_t"])
    k.act(sc_t, cc_t, AF.Silu, reads=["cc_t"], writes=["sc_t"])


    def phase_barrier(bump=True):
        P.barrier(bump)

    def norm_mod(xc, W, Acol, Scol, sq, lnv, rstd, tmp, out, out_is_f32, kx, kout, pz, pzk, scale_in=1.0 / D):
        k.act(sq[:, :, :W], xc[:, :, :W], AF.Square, reads=[kx], writes=["nm_sq"])
        for kc in range(8):
            k.mm(pz[:, :W], ones_bf, sq[:, kc, :W], start=(kc == 0), stop=(kc == 7),
                 reads=["nm_sq", "ones_bf"], writes=[pzk])
        k.act(lnv[:, :W], pz[:, :W], AF.Ln, bias=epsb[:, 0:1], scale=scale_in, reads=[pzk, "epsb"], writes=["nm_ln"])
        k.act(rstd[:, :W], lnv[:, :W], AF.Exp, scale=-0.5, reads=["nm_ln"], writes=["nm_rstd"])
        for kc in range(8):
            k.stt(tmp[:, kc, :W], xc[:, kc, :W], Acol[:, kc:kc + 1], rstd[:, :W], ALU.mult, ALU.mult,
                  reads=[kx, "nm_rstd", "modc"], writes=[("nm_tmp", kc)])
            k.act(out[:, kc, :W], tmp[:, kc, :W], AF.Identity, bias=Scol[:, kc:kc + 1], scale=1.0,
                  reads=[("nm_tmp", kc), "modc"], writes=[kout])

    stg_ctr = [0]

    def load_cast(stg, dst, src, key, np_=128, eng="pool"):
        Kk, cols = dst.shape[1], dst.shape[2]
        cb = max(1, int(stg[0].shape[1]) // Kk)
        for c0 in range(0, cols, cb):
            c1 = min(cols, c0 + cb)
            i = stg_ctr[0] % len(stg)
            stg_ctr[0] += 1
            sv = stg[i][0:np_, 0:Kk * (c1 - c0)].rearrange("p (k c) -> p k c", k=Kk)
            srcv = src[:, c0:c1].rearrange("(k p) c -> p k c", p=np_)
            k.dma(sv, srcv, writes=[("stg", i)])
            k.copy(eng, dst[0:np_, :, c0:c1], sv, reads=[("stg", i)], writes=[key])

    for l in range(NL):
        last = (l == NL - 1)
        phase_barrier()
        A.reset()
        k.dma(bm_t, bm[l], writes=["bm_t"])
        k.dma(g1_t, g1[l], writes=["g1_t"])
        k.dma(g2_t, g2[l], writes=["g2_t"])
        k.dma(gqk_t, gqk[l], writes=["gqk_t"])
        k.dma(gmq_t, gmq[l], writes=["gmq_t"])
        k.dma(gmkv_t, gmkv[l], writes=["gmkv_t"])
        k.dma(sink_t, sink[l], writes=["sink_t"])
        k.act(sink_t, sink_t, AF.Exp, reads=["sink_t"], writes=["sink_t"])
        wst = [A.tile(F32, [8, 512]) for _ in range(2)]
        for j in range(12):
            wv = wst[j % 2]
            k.dma(wv, w_mod[l][:, j * 512:(j + 1) * 512].rearrange("(k p) c -> p k c", p=128), writes=[("wst", j % 2)])
            for jj in range(4):
                f = j * 4 + jj
                for kc in range(8):
                    k.mm(ps[7][:, 2 * f:2 * f + 2], wv[:, kc, jj * 128:(jj + 1) * 128], sc3[:, kc, :],
                         start=(kc == 0), stop=(kc == 7), reads=[("wst", j % 2), "sc_t"], writes=[PSK[7]])
        k.tt("dve", modv3, ps[7][:, 0:96].rearrange("p (j c) -> p j c", c=2),
             bm_t.unsqueeze(2).broadcast_to([128, 48, 2]), ALU.add, reads=[PSK[7], "bm_t"], writes=["modc"])
        k.stt(A13, modv3[:, 8:16, :], 1.0, g1_t.unsqueeze(2).broadcast_to([128, 8, 2]), ALU.add, ALU.mult,
              reads=["modc", "g1_t"], writes=["modc"])
        k.stt(A23, modv3[:, 32:40, :], 1.0, g2_t.unsqueeze(2).broadcast_to([128, 8, 2]), ALU.add, ALU.mult,
              reads=["modc", "g2_t"], writes=["modc"])

        phase_barrier(False)
        A.reset()
        xb = [A.tile(F32, [8, 512]) for _ in range(2)]
        hb = [A.tile(BF16, [8, 512]) for _ in range(2)]
        sq = A.tile(BF16, [8, 512])
        tmp = A.tile(F32, [8, 512])
        lnv = A.tile(F32, [512])
        rstd = A.tile(F32, [512])
        chunks = list(range(9))
        for c in chunks:
            t0, W = CH[c]
            col = 1 if c == 8 else 0
            xc = xb[c % 2]
            k.dma(xc[:, :, :W], xsrc(l, c), writes=[("xb", c % 2)])
            norm_mod(xc, W, A13[:, :, col], modv3[:, 0:8, col], sq, lnv, rstd, tmp, hb[c % 2], False,
                     ("xb", c % 2), ("hb", c % 2), ps[6], PSK[6])
            k.dma(sview(HT[l], c), hb[c % 2][:, :, :W], reads=[("hb", c % 2)], writes=[("HT", c)])

        def rope_out(M, r0, W, a_ps, b_ps, ak, bk, Ct, St, tk, out_ap, wkey, t1, t2, gain=None, rr=None, t3=None, sx=0):
            k1, k2, k3, kr = ("t1", sx), ("t2", sx), ("t3", sx), ("rr", sx)
            if gain is None:
                k.tt("dve", t1[r0:M, :W], a_ps[r0:M, :W], Ct[r0:M, :W], ALU.mult, reads=[ak, tk], writes=[k1])
                k.tt("dve", t2[r0:M, :W], b_ps[r0:M, :W], St[r0:M, :W], ALU.mult, reads=[bk, tk], writes=[k2])
                k.tt("pool", out_ap, t1[r0:M, :W], t2[r0:M, :W], ALU.add, reads=[k1, k2, "KTz"], writes=[wkey])
            else:
                k.stt(t1[r0:M, :W], a_ps[r0:M, :W], gain[0], Ct[r0:M, :W], ALU.mult, ALU.mult,
                      reads=[ak, tk, "gqk_t"], writes=[k1])
                k.stt(t2[r0:M, :W], b_ps[r0:M, :W], gain[1], St[r0:M, :W], ALU.mult, ALU.mult,
                      reads=[bk, tk, "gqk_t"], writes=[k2])
                k.tt("pool", t3[r0:M, :W], t1[r0:M, :W], t2[r0:M, :W], ALU.add, reads=[k1, k2], writes=[k3])
                k.tt("pool", out_ap, t3[r0:M, :W], rr[r0:M, :W], ALU.mult, reads=[k3, kr, "KTz"], writes=[wkey])

        pend = [None]
        actr = [0]

        def flush_pend():
            if pend[0] is not None:
                fn_ = pend[0]
                pend[0] = None
                fn_()

        def attention(hg, dk, KTh, Vh, scale, typ, sink_idx, qb, ptb, den, rdenb, bcs, yst, msk, otsb):
            qchunks = list(range(8)) + ([8] if not last else [])
            ot, otk = ps[6], PSK[6]
            for c in qchunks:
                it = actr[0]
                actr[0] += 1
                t0, W = CH[c]
                qt = qb[it % 2]
                k.dma(qt[0:dk, :W], QS[l][hg, 0:dk, t0:t0 + W], reads=[("QS", hg, c)], writes=[("qb", it % 2)])
                if c == 8:
                    kbs = [(32, None), (33, None)]
                elif typ == "swa":
                    kbs = [(32, None), (33, None)] + [(j, j - 4 * c) for j in range(4 * c - 1, 4 * c + 5) if 0 <= j < 32]
                else:
                    kbs = [(j, None) for j in range(NKB)]
                groups = [kbs[i:i + 2] for i in range(0, len(kbs), 2)]

                def qk_group(gi):
                    b = (gi % 3) * 2
                    for j, (kb, rel) in enumerate(groups[gi]):
                        k.mm(ps[b + j][:, :W], KTh[:, kb * 128:(kb + 1) * 128], qt[:, :W],
                             reads=[("qb", it % 2)], writes=[PSK[b + j]])

                qk_group(0)
                if len(groups) > 1:
                    qk_group(1)
                n = 0
                for gi, g in enumerate(groups):
                    if gi + 2 < len(groups):
                        qk_group(gi + 2)
                    if gi == 1:
                        flush_pend()
                    b = (gi % 3) * 2
                    ng = len(g)
                    pt = ptb[gi % 3]
                    src = psbig[:, b * 512:(b + ng) * 512].rearrange("p (a t) -> p a t", a=ng)[:, :, :W]
                    k.act(pt[:, 0:ng, :W], src, AF.Exp, scale=scale, reads=[PSK[b + j] for j in range(ng)],
                          writes=[("pt", gi % 3)])
                    for j, (kb, rel) in enumerate(g):
                        if rel is not None:
                            k.tt("pool" if n % 2 == 0 else "dve", pt[:, j, :W], pt[:, j, :W], msk[:, rel + 1, :W], ALU.mult,
                                 reads=[("pt", gi % 3), "msk"], writes=[("pt", gi % 3)])
                        k.mm(ot[0:65, :W], Vh[:, kb, :], pt[:, j, :W], start=(n == 0), stop=(n == len(kbs) - 1),
                             reads=[("pt", gi % 3)], writes=[otk])
                        n += 1
                flush_pend()
                osb = otsb[it % 2]
                rd = rdenb[it % 2]
                k.copy("dve", osb[0:65, :W], ot[0:65, :W], reads=[otk], writes=[("otsb", it % 2)])
                if sink_idx is not None:
                    k.ts("dve", den[0:1, :W], osb[0:1, :W], sink_t[0:1, sink_idx:sink_idx + 1], ALU.add,
                         reads=[("otsb", it % 2), "sink_t"], writes=["den"])
                    k.recip(rd[0:1, :W], den[0:1, :W], reads=["den"], writes=[("rden", it % 2)])
                else:
                    k.recip(rd[0:1, :W], osb[0:1, :W], reads=[("otsb", it % 2)], writes=[("rden", it % 2)])

                def e2(osb=osb, rd=rd, it=it, hg=hg, t0=t0, W=W, c=c):
                    k.mm(ps[7][0:65, :W], ones_f[0:1, 0:65], rd[0:1, :W], reads=[("rden", it % 2), "ones_f"], writes=[PSK[7]])
                    k.act(bcs[0:65, :W], ps[7][0:65, :W], AF.Copy, reads=[PSK[7]], writes=["bcs"])
                    ys = yst[it % 2]
                    k.tt("dve", ys[0:65, :W], osb[0:65, :W], bcs[0:65, :W], ALU.mult, reads=[("otsb", it % 2), "bcs"],
                         writes=[("yst", it % 2)])
                    k.dma(YS[l][hg, :, t0:t0 + W], ys[0:65, :W], reads=[("yst", it % 2)], writes=[("YS", c)])

                pend[0] = e2

        for typ in ("swa", "glb", "mla"):
            phase_barrier()
            A.reset()
            nkv = 4 if typ == "mla" else 2
            dk = 96 if typ == "mla" else 64
            KT = A.tile(BF16, [nkv, NT])
            V = A.tile(BF16, [nkv, NKB, 65])
            mark = A.off
            stg = [A.tile(F32, [4096]) for _ in range(2)]
            hb = [A.tile(BF16, [8, 512]) for _ in range(2)]
            tb = [A.tile(F32, [2, 512]) for _ in range(2)]
            t1s = [A.tile(F32, [512]) for _ in range(2)]
            t2s = [A.tile(F32, [512]) for _ in range(2)]
            t3s = [A.tile(F32, [512]) for _ in range(2)]
            rrs = [A.tile(F32, [512]) for _ in range(2)]
            lnvs = [A.tile(F32, [512]) for _ in range(2)]
            sqhs = [A.tile(BF16, [2, 512]) for _ in range(2)]
            t1, t2, t3, rr, lnv, sqh = t1s[0], t2s[0], t3s[0], rrs[0], lnvs[0], sqhs[0]
            qst = [A.tile(BF16, [512]) for _ in range(2)]
            k.memset("pool", V[:, :, :, 0:1], 1.0, writes=["Vones"])
            k.memset("pool", KT, 0.0, writes=["KTz"])
            if typ != "mla":
                base = 0 if typ == "swa" else 640
                hbase = 0 if typ == "swa" else 6
                wq = A.tile(BF16, [8, 384])
                wqs = A.tile(BF16, [8, 384])
                wk = A.tile(BF16, [8, 128])
                wks = A.tile(BF16, [8, 128])
                wv = A.tile(BF16, [8, 128])
                load_cast(stg, wq, w_in[l][:, base:base + 384], "W")
                load_cast(stg, wqs, w_in_sw[l][:, base:base + 384], "W")
                load_cast(stg, wk, w_in[l][:, base + 384:base + 512], "W")
                load_cast(stg, wks, w_in_sw[l][:, base + 384:base + 512], "W")
                load_cast(stg, wv, w_in[l][:, base + 512:base + 640], "W")
                tix = 0
                for c in range(9):
                    t0, W = CH[c]
                    hc = hb[c % 2]
                    k.dma(hc[:, :, :W], sview(HT[l], c), reads=[("HT", c)], writes=[("hb", c % 2)])
                    tc_ = tb[c % 2]
                    k.dma(tc_[0:64, :, :W], tabs[0:2, 0:64, t0:t0 + W].rearrange("a p t -> p a t"), writes=[("tb", c % 2)])
                    Ct, St = tc_[:, 0, :], tc_[:, 1, :]
                    jobs = [("k", kv) for kv in range(2)]
                    if not (last and c == 8):
                        jobs += [("q", h) for h in range(6)]
                    for kind, idx in jobs:
                        sx = tix % 2
                        t1, t2, t3, rr, lnv, sqh = t1s[sx], t2s[sx], t3s[sx], rrs[sx], lnvs[sx], sqhs[sx]
                        wa, wb = (wk, wks) if kind == "k" else (wq, wqs)
                        pa, pb = ps[(tix % 2) * 2], ps[(tix % 2) * 2 + 1]
                        pak, pbk = PSK[(tix % 2) * 2], PSK[(tix % 2) * 2 + 1]
                        for kc in range(8):
                            k.mm(pa[0:64, :W], wa[:, kc, idx * 64:(idx + 1) * 64], hc[:, kc, :W], start=(kc == 0),
                                 stop=(kc == 7), reads=["W", ("hb", c % 2)], writes=[pak])
                        for kc in range(8):
                            k.mm(pb[0:64, :W], wb[:, kc, idx * 64:(idx + 1) * 64], hc[:, kc, :W], start=(kc == 0),
                                 stop=(kc == 7), reads=["W", ("hb", c % 2)], writes=[pbk])
                        if kind == "k":
                            out_ap, wkey = KT[0:64, idx, t0:t0 + W], ("KT", idx, c)
                        else:
                            out_ap, wkey = qst[tix % 2][0:64, :W], ("qst", tix % 2)
                        if typ == "glb":
                            go = 0 if kind == "q" else 2
                            k.act(sqh[0:64, 0, :W], pa[0:64, :W], AF.Square, reads=[pak], writes=[("sqh", sx)])
                            k.mm(ps[6][0:64, :W], ones_bf[0:64, 0:64], sqh[0:64, 0, :W], reads=[("sqh", sx), "ones_bf"], writes=[PSK[6]])
                            k.act(lnv[0:64, :W], ps[6][0:64, :W], AF.Ln, bias=epsb[0:64, 0:1], scale=1.0 / 64,
                                  reads=[PSK[6], "epsb"], writes=[("lnv", sx)])
                            k.act(rr[0:64, :W], lnv[0:64, :W], AF.Exp, scale=-0.5, reads=[("lnv", sx)], writes=[("rr", sx)])
                            rope_out(64, 0, W, pa, pb, pak, pbk, Ct, St, ("tb", c % 2), out_ap, wkey, t1, t2,
                                     gain=(gqk_t[:, go:go + 1], gqk_t[:, go + 1:go + 2]), rr=rr, t3=t3, sx=sx)
                        else:
                            rope_out(64, 0, W, pa, pb, pak, pbk, Ct, St, ("tb", c % 2), out_ap, wkey, t1, t2, sx=sx)
                        if kind == "q":
                            k.dma(QS[l][hbase + idx, 0:64, t0:t0 + W], out_ap, reads=[wkey], writes=[("QS", hbase + idx, c)])
                        tix += 1
                    for tt_ in range(W // 128):
                        kb = t0 // 128 + tt_
                        for kc in range(8):
                            k.mm(ps[5][:, 0:128], hc[:, kc, tt_ * 128:(tt_ + 1) * 128], wv[:, kc, :], start=(kc == 0),
                                 stop=(kc == 7), reads=["W", ("hb", c % 2)], writes=[PSK[5]])
                        k.copy("dve", V[:, :, kb, 1:65], ps[5][:, 0:128].rearrange("p (a d) -> p a d", a=2),
                               reads=[PSK[5], "Vones"], writes=[("V", kb)])
            else:
                wql = A.tile(BF16, [8, 256])
                wkvl = A.tile(BF16, [8, 128])
                wkr = A.tile(BF16, [8, 96])
                wkrs = A.tile(BF16, [8, 96])
                wuq = A.tile(BF16, [2, 384])
                wuqs = A.tile(BF16, [2, 384])
                wukv = A.tile(BF16, [1, 512])
                latq = A.tile(BF16, [2, 512])
                latkv = A.tile(BF16, [512])
                k.memset("pool", wkr, 0.0, writes=["W"])
                k.memset("pool", wkrs, 0.0, writes=["W"])
                load_cast(stg, wql, w_in[l][:, 1280:1536], "W")
                load_cast(stg, wkvl, w_in[l][:, 1536:1664], "W")
                load_cast(stg, wkr[:, :, 64:96], w_in[l][:, 1664:1696], "W")
                load_cast(stg, wkrs[:, :, 64:96], w_in_sw[l][:, 1664:1696], "W")
                load_cast(stg, wuq, w_uq[l], "W")
                load_cast(stg, wuqs, w_uq_sw[l], "W")
                load_cast(stg, wukv, w_ukv[l], "W")
                tix = 0
                for c in range(9):
                    t0, W = CH[c]
                    hc = hb[c % 2]
                    k.dma(hc[:, :, :W], sview(HT[l], c), reads=[("HT", c)], writes=[("hb", c % 2)])
                    tc_ = tb[c % 2]
                    k.dma(tc_[0:96, :, :W], tabs[2:4, 0:96, t0:t0 + W].rearrange("a p t -> p a t"), writes=[("tb", c % 2)])
                    Ct, St = tc_[:, 0, :], tc_[:, 1, :]
                    needq = not (last and c == 8)
                    if needq:
                        for kc2 in range(2):
                            for kc in range(8):
                                k.mm(ps[kc2][:, :W], wql[:, kc, kc2 * 128:(kc2 + 1) * 128], hc[:, kc, :W], start=(kc == 0),
                                     stop=(kc == 7), reads=["W", ("hb", c % 2)], writes=[PSK[kc2]])
                            k.act(sqh[:, kc2, :W], ps[kc2][:, :W], AF.Square, reads=[PSK[kc2]], writes=[("sqh", kc2)])
                        for kc2 in range(2):
                            k.mm(ps[6][:, :W], ones_bf, sqh[:, kc2, :W], start=(kc2 == 0), stop=(kc2 == 1),
                                 reads=[("sqh", kc2), "ones_bf"], writes=[PSK[6]])
                        k.act(lnv[:, :W], ps[6][:, :W], AF.Ln, bias=epsb[:, 0:1], scale=1.0 / 256, reads=[PSK[6], "epsb"], writes=["lnv"])
                        k.act(rr[:, :W], lnv[:, :W], AF.Exp, scale=-0.5, reads=["lnv"], writes=["rr"])
                        for kc2 in range(2):
                            k.stt(latq[:, kc2, :W], ps[kc2][:, :W], gmq_t[:, kc2:kc2 + 1], rr[:, :W], ALU.mult, ALU.mult,
                                  reads=[PSK[kc2], "rr", "gmq_t"], writes=["latq"])
                    for kc in range(8):
                        k.mm(ps[2][:, :W], wkvl[:, kc, :], hc[:, kc, :W], start=(kc == 0), stop=(kc == 7),
                             reads=["W", ("hb", c % 2)], writes=[PSK[2]])
                    k.act(sqh[:, 0, :W], ps[2][:, :W], AF.Square, reads=[PSK[2]], writes=[("sqh", 0)])
                    k.mm(ps[6][:, :W], ones_bf, sqh[:, 0, :W], reads=[("sqh", 0), "ones_bf"], writes=[PSK[6]])
                    k.act(lnv[:, :W], ps[6][:, :W], AF.Ln, bias=epsb[:, 0:1], scale=1.0 / 128, reads=[PSK[6], "epsb"], writes=["lnv"])
                    k.act(rr[:, :W], lnv[:, :W], AF.Exp, scale=-0.5, reads=["lnv"], writes=["rr"])
                    k.stt(latkv[:, :W], ps[2][:, :W], gmkv_t[:, 0:1], rr[:, :W], ALU.mult, ALU.mult,
                          reads=[PSK[2], "rr", "gmkv_t"], writes=["latkv"])
                    for kc in range(8):
                        k.mm(ps[0][0:96, :W], wkr[:, kc, :], hc[:, kc, :W], start=(kc == 0), stop=(kc == 7),
                             reads=["W", ("hb", c % 2)], writes=[PSK[0]])
                    for kc in range(8):
                        k.mm(ps[1][0:96, :W], wkrs[:, kc, :], hc[:, kc, :W], start=(kc == 0), stop=(kc == 7),
                             reads=["W", ("hb", c % 2)], writes=[PSK[1]])
                    rope_out(96, 64, W, ps[0], ps[1], PSK[0], PSK[1], Ct, St, ("tb", c % 2), KT[64:96, 0, t0:t0 + W], ("KTr", 0, c), t1, t2)
                    for h in range(1, 4):
                        k.copy("pool", KT[64:96, h, t0:t0 + W], KT[64:96, 0, t0:t0 + W], reads=[("KTr", 0, c)], writes=[("KTr", h, c)])
                    for h in range(4):
                        k.mm(ps[3][0:64, :W], wukv[:, 0, h * 128:h * 128 + 64], latkv[:, :W], reads=["W", "latkv"], writes=[PSK[3]])
                        k.act(KT[0:64, h, t0:t0 + W], ps[3][0:64, :W], AF.Copy, reads=[PSK[3], "KTz"], writes=[("KTn", h, c)])
                        if needq:
                            pa, pb = ps[(tix % 2) * 2], ps[(tix % 2) * 2 + 1]
                            pak, pbk = PSK[(tix % 2) * 2], PSK[(tix % 2) * 2 + 1]
                            for kc2 in range(2):
                                k.mm(pa[0:96, :W], wuq[:, kc2, h * 96:(h + 1) * 96], latq[:, kc2, :W], start=(kc2 == 0),
                                     stop=(kc2 == 1), reads=["W", "latq"], writes=[pak])
                            for kc2 in range(2):
                                k.mm(pb[0:96, :W], wuqs[:, kc2, h * 96:(h + 1) * 96], latq[:, kc2, :W], start=(kc2 == 0),
                                     stop=(kc2 == 1), reads=["W", "latq"], writes=[pbk])
                            out_ap, wkey = qst[tix % 2][0:96, :W], ("qst", tix % 2)
                            rope_out(96, 0, W, pa, pb, pak, pbk, Ct, St, ("tb", c % 2), out_ap, wkey, t1, t2)
                            k.dma(QS[l][12 + h, 0:96, t0:t0 + W], out_ap, reads=[wkey], writes=[("QS", 12 + h, c)])
                            tix += 1
                    wv4 = wukv[:, 0, :].rearrange("p (h d) -> p h d", h=4)[:, :, 64:128]
                    for tt_ in range(W // 128):
                        kb = t0 // 128 + tt_
                        k.mm(ps[5][:, 0:256].rearrange("p (h d) -> p h d", h=4), latkv[:, tt_ * 128:(tt_ + 1) * 128], wv4,
                             reads=["W", "latkv"], writes=[PSK[5]])
                        k.copy("dve", V[:, :, kb, 1:65], ps[5][:, 0:256].rearrange("p (a d) -> p a d", a=4),
                               reads=[PSK[5], "Vones"], writes=[("V", kb)])
            phase_barrier(typ == "glb")
            A.reset(mark)
            qb = [A.tile(BF16, [512]) for _ in range(2)]
            for i_ in range(2):
                k.memset("pool", qb[i_], 0.0, writes=[("qb", i_)])
            ptb = [A.tile(BF16, [2, 512]) for _ in range(3)]
            otsb = [A.tile(F32, [512]) for _ in range(2)]
            rdenb = [A.tile(F32, [512]) for _ in range(2)]
            yst = [A.tile(BF16, [512]) for _ in range(2)]
            den = A.tile(F32, [512])
            rden = A.tile(F32, [512])
            bcs = A.tile(F32, [512])
            msk = A.tile(BF16, [6, 512])
            if typ == "swa":
                mst = A.tile(F32, [6, 512])
                k.dma(mst, masks.rearrange("p (a t) -> p a t", a=6), writes=["mst"])
                k.copy("pool", msk, mst, reads=["mst"], writes=["msk"])
            nh = 4 if typ == "mla" else 6
            hbase = {"swa": 0, "glb": 6, "mla": 12}[typ]
            for h in range(nh):
                kvh = h if typ == "mla" else h // 3
                scale = (96 ** -0.5) if typ == "mla" else 0.125
                attention(hbase + h, dk, KT[:, kvh, :], V[:, kvh, :, :], scale, typ, h if typ == "swa" else None,
                          qb, ptb, den, rdenb, bcs, yst, msk, otsb)
            flush_pend()

        phase_barrier()
        A.reset()
        nch = 8 if last else 9
        stg = [A.tile(F32, [4096]) for _ in range(1)]
        wo = A.tile(BF16, [16, 1024])
        k.memset("pool", wo, 0.0, writes=["wo"])
        for hh in range(0, 16, 4):
            sv = stg[0][:, 0:4096].rearrange("p (h c) -> p h c", h=4)
            k.memset("pool", sv[0:1, :, :], 0.0, writes=[("stg", 0)])
            k.dma(sv[1:65, :, :], w_out[l][hh * 64:(hh + 4) * 64, :].rearrange("(h r) c -> r h c", r=64), writes=[("stg", 0)])
            k.copy("pool", wo[0:65, hh:hh + 4, :], sv[0:65, :, :], reads=[("stg", 0)], writes=["wo"])
        ytb = [A.tile(BF16, [16, 512]) for _ in range(1)]
        k.memset("pool", ytb[0], 0.0, writes=[("ytb", 0)])
        xb = [A.tile(F32, [8, 512]) for _ in range(2)]
        x1bs = [A.tile(F32, [8, 512]) for _ in range(2)]
        h32 = A.tile(F32, [8, 512])
        h2b = [A.tile(BF16, [8, 512]) for _ in range(2)]
        sq = A.tile(BF16, [8, 512])
        tmp = A.tile(F32, [8, 512])
        lnv = A.tile(F32, [512])
        rstd = A.tile(F32, [512])
        tile_i = 0
        for c in range(nch):
            t0, W = CH[c]
            col = 1 if c == 8 else 0
            yt = ytb[0]
            x1b = x1bs[c % 2]
            x1k = ("x1b", c % 2)
            k.dma(yt[0:65, :, :W], YS[l][:, :, t0:t0 + W].rearrange("h r t -> r h t"), reads=[("YS", c)], writes=[("ytb", 0)])
            xc = xb[c % 2]
            k.dma(xc[:, :, :W], xsrc(l, c), writes=[("xb", c % 2)])
            for dc in range(8):
                po, pok = ps[dc % 2], PSK[dc % 2]
                for hh in range(16):
                    k.mm(po[:, :W], wo[:, hh, dc * 128:(dc + 1) * 128], yt[:, hh, :W], start=(hh == 0), stop=(hh == 15),
                         reads=["wo", ("ytb", 0)], writes=[pok])
                k.stt(x1b[:, dc, :W], po[:, :W], modv3[:, 16 + dc, col:col + 1], xc[:, dc, :W], ALU.mult, ALU.add,
                      reads=[pok, ("xb", c % 2), "modc"], writes=[x1k])
            k.dma(sview(X1[l], c), x1b[:, :, :W], reads=[x1k], writes=[("X1", c)])
            norm_mod(x1b, W, A23[:, :, col], modv3[:, 24:32, col], sq, lnv, rstd, tmp, h32, True,
                     x1k, "h32", ps[6], PSK[6])
            h2 = h2b[c % 2]
            k.copy("pool", h2[:, :, :W], h32[:, :, :W], reads=["h32"], writes=[("h2b", c % 2)])
            k.dma(sview(HT2[l], c), h2[:, :, :W], reads=[("h2b", c % 2)], writes=[("HT2", c)])
            for tt_ in range(W // 128):
                R = rt[tile_i % 2]
                rk = ("rt", tile_i % 2)
                lg = ps[4 + tile_i % 2][:, 0:16]
                lgk = PSK[4 + tile_i % 2]
                for kc in range(8):
                    k.mm(lg, h32[:, kc, tt_ * 128:(tt_ + 1) * 128], rw3[:, kc, :], start=(kc == 0), stop=(kc == 7),
                         reads=["h32", "rw_t"], writes=[lgk])
                e1, scv, bs, eq, b2, ge, selv, gu, G = [R[:, i * 16:(i + 1) * 16] for i in range(9)]
                m1, m2, gs, gsel = [R[:, 144 + i * 4:148 + i * 4] for i in range(4)]
                v3 = lambda ap: ap.rearrange("p (g e) -> p g e", e=4)
                bc4 = lambda ap: ap.unsqueeze(2).broadcast_to([128, 4, 4])
                k.act(e1, lg, AF.Exp, scale=-1.0, reads=[lgk], writes=[rk])
                k.ts("dve", e1, e1, 1.0, ALU.add, reads=[rk], writes=[rk])
                k.recip(scv, e1, reads=[rk], writes=[rk])
                k.tt("dve", bs, scv, rb_t, ALU.add, reads=[rk, "rb_t"], writes=[rk])
                k.reduce(m1, v3(bs), ALU.max, AX.X, reads=[rk], writes=[rk])
                k.tt("dve", v3(eq), v3(bs), bc4(m1), ALU.is_equal, reads=[rk], writes=[rk])
                k.stt(v3(b2), v3(eq), -1e9, v3(bs), ALU.mult, ALU.add, reads=[rk], writes=[rk])
                k.reduce(m2, v3(b2), ALU.max, AX.X, reads=[rk], writes=[rk])
                k.tt("dve", gs, m1, m2, ALU.add, reads=[rk], writes=[rk])
                k.reduce(e1[:, 0:1], gs, ALU.max, AX.X, reads=[rk], writes=[rk])
                k.ts("dve", gsel, gs, e1[:, 0:1], ALU.is_equal, reads=[rk], writes=[rk])
                k.tt("dve", v3(ge), v3(bs), bc4(m2), ALU.is_ge, reads=[rk], writes=[rk])
                k.tt("dve", v3(selv), v3(ge), bc4(gsel), ALU.mult, reads=[rk], writes=[rk])
                k.tt("dve", gu, scv, selv, ALU.mult, reads=[rk], writes=[rk])
                k.reduce(e1[:, 1:2], gu, ALU.add, AX.X, reads=[rk], writes=[rk])
                k.recip(e1[:, 2:3], e1[:, 1:2], reads=[rk], writes=[rk])
                tok_tile = t0 // 128 + tt_
                k.ts("dve", Gs3[:, tok_tile, :], gu, e1[:, 2:3], ALU.mult, reads=[rk], writes=[("Gs", tok_tile)])
                tile_i += 1

        phase_barrier()
        A.reset()
        scs = [[0, 1, 2], [3, 4, 5], [6, 7] if last else [6, 7, 8]]
        stg = [A.tile(F32, [2048]) for _ in range(3)]
        diag = A.tile(F32, [4, 128])
        wgb = [A.tile(BF16, [8, 512]) for _ in range(2)]
        wub = [A.tile(BF16, [8, 512]) for _ in range(2)]
        wdb = [A.tile(BF16, [4, 1024]) for _ in range(2)]
        h2b = [A.tile(BF16, [8, 512]) for _ in range(2)]
        actb = [A.tile(BF16, [4, 512]) for _ in range(2)]
        acc = A.tile(F32, [8, 1536])
        bcs = A.tile(F32, [512])
        sgb = [A.tile(F32, [512]) for _ in range(2)]
        ugb = [A.tile(BF16, [512]) for _ in range(2)]
        x1c = A.tile(F32, [8, 512])
        sq = A.tile(BF16, [8, 512])
        lnv = A.tile(F32, [512])
        rstd = A.tile(F32, [512])
        items = [(si, e) for si in range(len(scs)) for e in range(17)]
        steps = []
        for n_, (si, e) in enumerate(items):
            o = 0
            for sidx, c in enumerate(scs[si]):
                steps.append(dict(n=n_, si=si, e=e, c=c, sidx=sidx, nsteps=len(scs[si]), k=len(steps), off=o))
                o += CH[c][1]

        def piece(n_, p):
            si, e = items[n_]
            b = n_ % 2
            srcs = (ewg[l, e], ewu[l, e], ewd[l, e]) if e < 16 else (swg[l], swu[l], swd[l])
            if p < 2:
                dst = wgb[b][:, :, p * 256:(p + 1) * 256]
                src = srcs[0][:, p * 256:(p + 1) * 256].rearrange("(k p) c -> p k c", p=128)
                Kk = 8
            elif p < 4:
                q = p - 2
                dst = wub[b][:, :, q * 256:(q + 1) * 256]
                src = srcs[1][:, q * 256:(q + 1) * 256].rearrange("(k p) c -> p k c", p=128)
                Kk = 8
            else:
                q = p - 4
                dst = wdb[b][:, :, q * 512:(q + 1) * 512]
                src = srcs[2][:, q * 512:(q + 1) * 512].rearrange("(k p) c -> p k c", p=128)
                Kk = 4
            sv = stg[p % 3].rearrange("p (k c) -> p k c", k=Kk)
            return dst, src, sv, ("wexp", b, p)

        def piece_dma(n_, p):
            dst, src, sv, key = piece(n_, p)
            k.dma(sv, src, writes=[("stg", p % 3)])

        def piece_cast(n_, p):
            dst, src, sv, key = piece(n_, p)
            k.act(dst, sv, AF.Copy, reads=[("stg", p % 3)], writes=[key])

        def h2_dma(st):
            t0, W = CH[st["c"]]
            kk = st["k"]
            k.dma(h2b[kk % 2][:, :, :W], sview(HT2[l], st["c"]), reads=[("HT2", st["c"])], writes=[("h2b", kk % 2)])

        def gu_part(st):
            n_, e, c, sidx, nsteps, kk = st["n"], st["e"], st["c"], st["sidx"], st["nsteps"], st["k"]
            t0, W = CH[c]
            b = n_ % 2
            wg, wu = wgb[b], wub[b]
            pre = n_ + 1 < len(items)
            if pre:
                if nsteps == 3:
                    if sidx == 0:
                        for p in (0, 1, 2):
                            piece_dma(n_ + 1, p)
                    elif sidx == 1:
                        for p in (0, 1, 2):
                            piece_cast(n_ + 1, p)
                        for p in (3, 4, 5):
                            piece_dma(n_ + 1, p)
                    else:
                        for p in (3, 4, 5):
                            piece_cast(n_ + 1, p)
                else:
                    for p in ((0, 1, 2) if sidx == 0 else (3, 4, 5)):
                        piece_dma(n_ + 1, p)
            if kk + 1 < len(steps):
                h2_dma(steps[kk + 1])
            h2 = h2b[kk % 2]
            hk = ("h2b", kk % 2)
            if e < 16:
                for tt_ in range(W // 128):
                    tok_tile = t0 // 128 + tt_
                    k.ts("dve", diag[:, tt_, :], ident, Gs3[:, tok_tile, e:e + 1], ALU.mult,
                         reads=["ident"], writes=[("diag", tt_)])
                    k.mm(ps[6][:, tt_ * 128:(tt_ + 1) * 128], ones_f, diag[:, tt_, :],
                         reads=[("diag", tt_), "ones_f"], writes=[PSK[6]])
                k.act(bcs[:, :W], ps[6][:, :W], AF.Copy, reads=[PSK[6]], writes=["bcs"])
            act_t = actb[kk % 2]
            ak = ("act", kk % 2)
            for fc in range(4):
                pg, pgk = ps[fc % 2], PSK[fc % 2]
                pu, puk = ps[2 + fc % 2], PSK[2 + fc % 2]
                wgk = ("wexp", b, 0 if fc < 2 else 1)
                wuk = ("wexp", b, 2 if fc < 2 else 3)
                for kc in range(8):
                    k.mm(pg[:, :W], wg[:, kc, fc * 128:(fc + 1) * 128], h2[:, kc, :W], start=(kc == 0), stop=(kc == 7),
                         reads=[wgk, hk], writes=[pgk])
                for kc in range(8):
                    k.mm(pu[:, :W], wu[:, kc, fc * 128:(fc + 1) * 128], h2[:, kc, :W], start=(kc == 0), stop=(kc == 7),
                         reads=[wuk, hk], writes=[puk])
                sg, ug = sgb[fc % 2], ugb[fc % 2]
                k.act(sg[:, :W], pg[:, :W], AF.Silu, reads=[pgk], writes=[("sg", fc % 2)])
                if e < 16:
                    k.tt("dve", ug[:, :W], pu[:, :W], bcs[:, :W], ALU.mult, reads=[puk, "bcs"], writes=[("ug", fc % 2)])
                else:
                    k.copy("dve", ug[:, :W], pu[:, :W], reads=[puk], writes=[("ug", fc % 2)])
                k.tt("pool", act_t[:, fc, :W], sg[:, :W], ug[:, :W], ALU.mult,
                     reads=[("sg", fc % 2), ("ug", fc % 2)], writes=[ak])
            if pre and nsteps != 3:
                for p in ((0, 1, 2) if sidx == 0 else (3, 4, 5)):
                    piece_cast(n_ + 1, p)

        def down_part(st):
            n_, e, c, sidx, kk, off = st["n"], st["e"], st["c"], st["sidx"], st["k"], st["off"]
            t0, W = CH[c]
            b = n_ % 2
            wd = wdb[b]
            act_t = actb[kk % 2]
            ak = ("act", kk % 2)
            for dc in range(8):
                py, pyk = ps[4 + dc % 2], PSK[4 + dc % 2]
                wdk = ("wexp", b, 4 if dc < 4 else 5)
                for fc in range(4):
                    k.mm(py[:, :W], wd[:, fc, dc * 128:(dc + 1) * 128], act_t[:, fc, :W], start=(fc == 0), stop=(fc == 3),
                         reads=[wdk, ak], writes=[pyk])
                av = acc[:, dc, off:off + W]
                if e == 0:
                    k.copy("dve", av, py[:, :W], reads=[pyk], writes=[("acc", sidx, dc)])
                else:
                    k.tt("dve", av, py[:, :W], av, ALU.add, reads=[pyk, ("acc", sidx, dc)], writes=[("acc", sidx, dc)])

        def epilogue(si):
            o = 0
            for sidx, c in enumerate(scs[si]):
                t0, W = CH[c]
                col = 1 if c == 8 else 0
                off = o
                o += W
                k.dma(x1c[:, :, :W], sview(X1[l], c), reads=[("X1", c)], writes=["x1c"])
                for dc in range(8):
                    k.stt(x1c[:, dc, :W], acc[:, dc, off:off + W], modv3[:, 40 + dc, col:col + 1], x1c[:, dc, :W],
                          ALU.mult, ALU.add, reads=[("acc", sidx, dc), "x1c", "modc"], writes=["x1c"])
                if not last:
                    k.dma(sview(X2[l], c), x1c[:, :, :W], reads=["x1c"], writes=[("X2", c)])
                else:
                    k.act(sq[:, :, :W], x1c[:, :, :W], AF.Square, reads=["x1c"], writes=["fsq"])
                    for kc in range(8):
                        k.mm(ps[7][:, :W], ones_bf, sq[:, kc, :W], start=(kc == 0), stop=(kc == 7), reads=["fsq", "ones_bf"], writes=[PSK[7]])
                    k.act(lnv[:, :W], ps[7][:, :W], AF.Ln, bias=epsb[:, 0:1], scale=1.0 / D, reads=[PSK[7], "epsb"], writes=["flnv"])
                    k.act(rstd[:, :W], lnv[:, :W], AF.Exp, scale=-0.5, reads=["flnv"], writes=["frstd"])
                    for kc in range(8):
                        k.stt(acc[:, kc, off:off + W], x1c[:, kc, :W], gf_t[:, kc:kc + 1], rstd[:, :W], ALU.mult, ALU.mult,
                              reads=["x1c", "frstd", "gf_t", ("acc", sidx, kc)], writes=[("acc", sidx, kc)])
                    k.dma(outT[:, t0:t0 + W].rearrange("(k p) t -> p k t", p=128), acc[:, :, off:off + W],
                          reads=[("acc", sidx, kc) for kc in range(8)], writes=[("out", c)])

        for p in range(6):
            piece_dma(0, p)
            piece_cast(0, p)
        h2_dma(steps[0])
        gu_part(steps[0])
        for kk in range(len(steps)):
            st = steps[kk]
            if kk + 1 < len(steps):
                gu_part(steps[kk + 1])
            down_part(st)
            if st["e"] == 16 and st["sidx"] == st["nsteps"] - 1:
                epilogue(st["si"])

    P.emit()
    return nc


def _rope_tables():
    theta = 10000.0
    t = np.arange(SEQ)
    rows = (t // 64).astype(np.float64)
    cols = (t % 64).astype(np.float64)
    tabs = np.zeros((4, 128, NT), np.float32)
    tabs[0, :, :] = 1.0
    tabs[2, :, :] = 1.0

    def fill(ci, si, r0, nf):
        inv = theta ** (-np.arange(nf, dtype=np.float64) / nf)
        for half, pos in enumerate((rows, cols)):
            ang = pos[None, :] * inv[:, None]
            b = r0 + half * 2 * nf
            tabs[ci, b:b + nf, :SEQ] = np.cos(ang)
            tabs[ci, b + nf:b + 2 * nf, :SEQ] = np.cos(ang)
            tabs[si, b:b + nf, :SEQ] = -np.sin(ang)
            tabs[si, b + nf:b + 2 * nf, :SEQ] = np.sin(ang)

    fill(0, 1, 0, 16)
    fill(2, 3, 64, 8)
    return tabs


def _swap_perm(n, nf):
    p = np.arange(n)
    out = p.copy()
    for b in range(0, n, 2 * nf):
        out[b:b + nf] = p[b + nf:b + 2 * nf]
        out[b + nf:b + 2 * nf] = p[b:b + nf]
    return out


def _masks():
    a = np.arange(128)
    lo = (a[None, :] <= a[:, None]).astype(np.float32)
    hi = (a[None, :] >= a[:, None]).astype(np.float32)
    one = np.ones((128, 128), np.float32)
    zero = np.zeros((128, 128), np.float32)
    m = np.zeros((128, 6, 512), np.float32)
    for r in range(6):
        rel = r - 1
        for i in range(4):
            d = rel - i
            blk = one if d == 0 else lo if d == -1 else hi if d == 1 else zero
            m[:, r, i * 128:(i + 1) * 128] = blk
    return m.reshape(128, 6 * 512)


_NC_CACHE = {}


def kernel(x, c, ctx, c_ctx, w_mod, b_mod, norm_mix_g, norm_ffn_g, w_in, w_out, swa_sink,
           glb_q_gain, glb_k_gain, mla_q_gain, mla_w_uq, mla_kv_gain, mla_w_ukv,
           router_w, router_bias, exp_w_gate, exp_w_up, exp_w_down,
           shr_w_gate, shr_w_up, shr_w_down, final_norm_g, _dbg=False, _cores=None):
    f = lambda a: np.ascontiguousarray(np.asarray(a, dtype=np.float32))
    x, c, ctx, c_ctx = f(x), f(c), f(ctx), f(c_ctx)
    w_in = f(w_in)
    perm = np.arange(1696)
    p64 = _swap_perm(64, 16)
    for hb_ in list(range(0, 640, 64)) + list(range(640, 1280, 64)):
        perm[hb_:hb_ + 64] = hb_ + p64
    perm[1664:1696] = 1664 + _swap_perm(32, 8)
    w_in_sw = np.ascontiguousarray(w_in[:, :, perm])
    w_uq = f(mla_w_uq)
    pu = np.arange(384)
    for h in range(4):
        pu[h * 96 + 64:h * 96 + 96] = h * 96 + 64 + _swap_perm(32, 8)
    w_uq_sw = np.ascontiguousarray(w_uq[:, :, pu])
    gq, gk = f(glb_q_gain), f(glb_k_gain)
    gqk = np.stack([gq, gq[:, p64], gk, gk[:, p64]], axis=-1)
    gmq = np.ascontiguousarray(f(mla_q_gain).reshape(NL, 2, 128).transpose(0, 2, 1))
    gmkv = f(mla_kv_gain).reshape(NL, 128, 1)
    bm = np.ascontiguousarray(f(b_mod).reshape(NL, 48, 128).transpose(0, 2, 1))
    g1 = np.ascontiguousarray(f(norm_mix_g).reshape(NL, 8, 128).transpose(0, 2, 1))
    g2 = np.ascontiguousarray(f(norm_ffn_g).reshape(NL, 8, 128).transpose(0, 2, 1))
    gfin = np.ascontiguousarray(f(final_norm_g).reshape(8, 128).T)
    rw = np.ascontiguousarray(f(router_w).reshape(8, 128, 16).transpose(1, 0, 2).reshape(128, 128))
    shared = {
        "w_mod": f(w_mod), "bm": bm, "g1": g1, "g2": g2, "gf": gfin, "w_in": w_in, "w_in_sw": w_in_sw,
        "w_out": f(w_out), "sink": f(swa_sink).reshape(NL, 1, 6), "gqk": np.ascontiguousarray(gqk), "gmq": gmq, "gmkv": gmkv,
        "w_uq": w_uq, "w_uq_sw": w_uq_sw, "w_ukv": f(mla_w_ukv), "rw": rw, "rb": f(router_bias),
        "ewg": f(exp_w_gate), "ewu": f(exp_w_up), "ewd": f(exp_w_down),
        "swg": f(shr_w_gate), "swu": f(shr_w_up), "swd": f(shr_w_down),
        "tabs": _rope_tables(), "masks": _masks(), "ident": np.eye(128, dtype=np.float32),
    }
    cores = list(range(8)) if _cores is None else _cores
    in_maps = []
    for b in cores:
        m = dict(shared)
        m["xT"] = np.ascontiguousarray(x[b].T)
        m["ctxT"] = np.ascontiguousarray(ctx[b].T)
        ccv = np.stack([c[b], c_ctx], axis=-1).reshape(8, 128, 2).transpose(1, 0, 2).reshape(128, 16)
        m["cc"] = np.ascontiguousarray(ccv)
        in_maps.append(m)
    key = bool(_dbg)
    if key not in _NC_CACHE:
        _NC_CACHE[key] = build_program(dbg=_dbg)
    nc = _NC_CACHE[key]
    res = run_bass_kernel_spmd(nc, in_maps, core_ids=list(range(len(cores))))
    if _dbg:
        return res
    out = np.stack([np.ascontiguousarray(r["outT"].T) for r in res.results], axis=0)
    return out.astype(np.float32)
```

```python
import numpy as np
import concourse.bass as bass
import concourse.mybir as mybir
from concourse.bass_utils import run_bass_kernel_spmd

F32 = mybir.dt.float32
BF16 = mybir.dt.bfloat16
I32 = mybir.dt.int32
U32 = mybir.dt.uint32
ALU = mybir.AluOpType
AF = mybir.ActivationFunctionType

ENGS = ("pe", "dve", "act", "pool", "sp")
NDMASEM = 24


class Op:
    __slots__ = ("eng", "fn", "deps", "needs_inc", "val", "dma", "dsem", "dval", "idx", "epoch")

    def __init__(self, eng, fn, dma=False):
        self.eng = eng
        self.fn = fn
        self.deps = []
        self.needs_inc = False
        self.val = 0
        self.dma = dma
        self.dsem = None
        self.dval = 0
        self.idx = 0
        self.epoch = 0


class Prog:
    def __init__(self, nc):
        self.nc = nc
        self.ops = {e: [] for e in ENGS}
        self.last_w = {}
        self.readers = {}
        self.ndma = 0
        self.dma_prev = [None] * NDMASEM
        self.all_ops = []
        self.epoch = 0

    def _add(self, eng, fn, reads, writes, dma=False):
        op = Op(eng, fn, dma)
        op.idx = len(self.ops[eng])
        op.epoch = self.epoch
        deps = []
        for k in reads:
            w = self.last_w.get(k)
            if w is not None:
                deps.append(w)
            if isinstance(k, tuple) and k and k[0] == "ps":
                for r in self.readers.get(k, ()):
                    if r.eng != eng:
                        deps.append(r)
        for k in writes:
            w = self.last_w.get(k)
            if w is not None:
                deps.append(w)
            for r in self.readers.get(k, ()):
                if r.eng != eng or r.dma or dma:
                    deps.append(r)
        if dma:
            s = self.ndma % NDMASEM
            self.ndma += 1
            prev = self.dma_prev[s]
            op.dsem = s
            op.dval = (prev.dval if prev is not None else 0) + 16
            if prev is not None:
                deps.append(prev)
            self.dma_prev[s] = op
        seen = set()
        for d in deps:
            if d is op or id(d) in seen:
                continue
            seen.add(id(d))
            if (not d.dma) and d.eng == eng and eng == "pe" and not dma:
                continue
            op.deps.append(d)
            if not d.dma:
                d.needs_inc = True
        for k in reads:
            self.readers.setdefault(k, []).append(op)
        for k in writes:
            self.last_w[k] = op
            self.readers[k] = []
        self.ops[eng].append(op)
        self.all_ops.append(op)
        return op

    def pe(self, fn, reads=(), writes=()):
        return self._add("pe", fn, reads, writes)

    def dve(self, fn, reads=(), writes=()):
        return self._add("dve", fn, reads, writes)

    def act(self, fn, reads=(), writes=()):
        return self._add("act", fn, reads, writes)

    def pool(self, fn, reads=(), writes=()):
        return self._add("pool", fn, reads, writes)

    def dma(self, fn, reads=(), writes=(), q="sp"):
        return self._add(q, fn, reads, writes, dma=True)

    def barrier(self, bump=True):
        key = ("__barrier__", len(self.all_ops))
        lasts = []
        for e in ENGS:
            for op in reversed(self.ops[e]):
                if (not op.dma) and op.fn is not None:
                    lasts.append(op)
                    break
        for d in self.dma_prev:
            if d is not None:
                lasts.append(d)
        self._barrier_deps = lasts
        self.pending_barrier = {e: list(lasts) for e in ENGS}
        for e in ENGS:
            op = Op(e, None, False)
            op.idx = len(self.ops[e])
            for d in lasts:
                if d.eng == e and not d.dma:
                    continue
                op.deps.append(d)
                if not d.dma:
                    d.needs_inc = True
            self.ops[e].append(op)
        self.last_w = {}
        self.readers = {}
        if bump:
            self.epoch += 1

    def emit(self, final_waits=True):
        nc = self.nc
        used = set()
        for e in ENGS:
            c = {}
            for op in self.ops[e]:
                if op.dma:
                    continue
                if op.needs_inc:
                    c[op.epoch] = c.get(op.epoch, 0) + 1
                    op.val = c[op.epoch]
                    used.add((e, op.epoch))
        import contextlib

        with contextlib.ExitStack() as st:
            esem = {(e, ep): st.enter_context(nc.semaphore("s_%s_%d" % (e, ep))) for (e, ep) in sorted(used)}
            dsem = [st.enter_context(nc.semaphore("d_%d" % i)) for i in range(NDMASEM)]
            block = st.enter_context(nc.Block())

            def run(e, eng):
                waited = {}
                for op in self.ops[e]:
                    for d in op.deps:
                        if d.dma:
                            key, val, sem = ("d", d.dsem), d.dval, dsem[d.dsem]
                        else:
                            key, val, sem = (d.eng, d.epoch), d.val, esem[(d.eng, d.epoch)]
                        if waited.get(key, 0) >= val:
                            continue
                        eng.wait_ge(sem, val)
                        waited[key] = val
                    if op.fn is None:
                        continue
                    ins = op.fn(eng)
                    if op.dma:
                        ins.then_inc(dsem[op.dsem], 16)
                    elif op.needs_inc:
                        ins.then_inc(esem[(e, op.epoch)], 1)
                if final_waits:
                    for d in self.dma_prev:
                        if d is not None and d.eng == e:
                            if waited.get(("d", d.dsem), 0) < d.dval:
                                eng.wait_ge(dsem[d.dsem], d.dval)

            @block.tensor
            def _(eng):
                run("pe", eng)

            @block.vector
            def _(eng):
                run("dve", eng)

            @block.scalar
            def _(eng):
                run("act", eng)

            @block.gpsimd
            def _(eng):
                run("pool", eng)

            @block.sync
            def _(eng):
                run("sp", eng)


AX = mybir.AxisListType
D = 1024
SEQ = 4096
NCTX = 256
NT = SEQ + NCTX
NL = 2
CH = [(i * 512, 512) for i in range(8)] + [(4096, 256)]
NKB = NT // 128
EPS = 1e-6
ARENA_WORDS = 48500
DEBUG = False


class Arena:
    def __init__(self, nc, words):
        self.t = nc.alloc_sbuf_tensor("arena", [128, words], F32).ap()
        self.words = words
        self.off = 0

    def reset(self, off=0):
        self.off = off

    def tile(self, dtype, shape):
        n = int(np.prod(shape))
        nbytes = n * (2 if dtype == BF16 else 4)
        w = (nbytes + 3) // 4
        w = (w + 7) // 8 * 8
        o = self.off
        self.off += w
        assert self.off <= self.words, "arena overflow %d" % self.off
        v = self.t[:, o:o + w]
        if dtype != F32:
            v = v.bitcast(dtype)
        v = v[:, 0:n]
        if len(shape) == 2:
            v = v.rearrange("p (a b) -> p a b", a=shape[0])
        elif len(shape) == 3:
            v = v.rearrange("p (a b c) -> p a b c", a=shape[0], b=shape[1])
        return v


class K:
    def __init__(self, P):
        self.P = P

    def mm(self, out, lhsT, rhs, start=True, stop=True, reads=(), writes=()):
        return self.P.pe(lambda e: e.matmul(out, lhsT=lhsT, rhs=rhs, start=start, stop=stop), reads, writes)

    def transpose(self, out, in_, ident, reads=(), writes=()):
        return self.P.pe(lambda e: e.transpose(out, in_, ident), reads, writes)

    def act(self, out, in_, func, bias=None, scale=1.0, reads=(), writes=()):
        if bias is None:
            return self.P.act(lambda e: e.activation(out=out, in_=in_, func=func, scale=scale), reads, writes)
        return self.P.act(lambda e: e.activation(out=out, in_=in_, func=func, bias=bias, scale=scale), reads, writes)

    def _eng(self, eng):
        return {"dve": self.P.dve, "pool": self.P.pool}[eng]

    def tt(self, eng, out, in0, in1, op, reads=(), writes=()):
        return self._eng(eng)(lambda e: e.tensor_tensor(out=out, in0=in0, in1=in1, op=op), reads, writes)

    def ts(self, eng, out, in0, s1, op0, s2=None, op1=None, reads=(), writes=()):
        if op1 is None:
            return self._eng(eng)(lambda e: e.tensor_scalar(out=out, in0=in0, scalar1=s1, scalar2=None, op0=op0), reads, writes)
        return self._eng(eng)(lambda e: e.tensor_scalar(out=out, in0=in0, scalar1=s1, scalar2=s2, op0=op0, op1=op1), reads, writes)

    def stt(self, out, in0, scalar, in1, op0, op1, reads=(), writes=()):
        return self.P.dve(lambda e: e.scalar_tensor_tensor(out=out, in0=in0, scalar=scalar, in1=in1, op0=op0, op1=op1), reads, writes)

    def copy(self, eng, out, in_, reads=(), writes=()):
        return self._eng(eng)(lambda e: e.tensor_copy(out=out, in_=in_), reads, writes)

    def recip(self, out, in_, reads=(), writes=()):
        return self.P.dve(lambda e: e.reciprocal(out=out, in_=in_), reads, writes)

    def reduce(self, out, in_, op, axis, reads=(), writes=()):
        return self.P.dve(lambda e: e.tensor_reduce(out=out, in_=in_, axis=axis, op=op), reads, writes)

    def memset(self, eng, ap, val, writes=()):
        return self._eng(eng)(lambda e: e.memset(ap, val), (), writes)

    def dma(self, out, in_, reads=(), writes=(), q="sp"):
        return self.P.dma(lambda e: e.dma_start(out=out, in_=in_), reads, writes, q=q)


def build_program(dbg=False):
    nc = bass.Bass("TRN2", target_bir_lowering=False)
    P = Prog(nc)
    k = K(P)

    def din(name, shape, dt=F32):
        return nc.dram_tensor(name, list(shape), dt, kind="ExternalInput").ap()

    def dscr(name, shape, dt):
        if dbg:
            return nc.dram_tensor(name, list(shape), dt, kind="ExternalOutput").ap()
        return nc.dram_tensor(name, list(shape), dt).ap()

    xT = din("xT", [D, SEQ])
    ctxT = din("ctxT", [D, NCTX])
    cc = din("cc", [128, 16])
    w_mod = din("w_mod", [NL, D, 6 * D])
    bm = din("bm", [NL, 128, 48])
    g1 = din("g1", [NL, 128, 8])
    g2 = din("g2", [NL, 128, 8])
    gf = din("gf", [128, 8])
    w_in = din("w_in", [NL, D, 1696])
    w_in_sw = din("w_in_sw", [NL, D, 1696])
    w_out = din("w_out", [NL, D, D])
    sink = din("sink", [NL, 1, 6])
    gqk = din("gqk", [NL, 64, 4])
    gmq = din("gmq", [NL, 128, 2])
    gmkv = din("gmkv", [NL, 128, 1])
    w_uq = din("w_uq", [NL, 256, 384])
    w_uq_sw = din("w_uq_sw", [NL, 256, 384])
    w_ukv = din("w_ukv", [NL, 128, 512])
    rw = din("rw", [128, 8 * 16])
    rb = din("rb", [16])
    ewg = din("ewg", [NL, 16, D, 512])
    ewu = din("ewu", [NL, 16, D, 512])
    ewd = din("ewd", [NL, 16, 512, D])
    swg = din("swg", [NL, D, 512])
    swu = din("swu", [NL, D, 512])
    swd = din("swd", [NL, 512, D])
    tabs = din("tabs", [4, 128, NT])
    masks = din("masks", [128, 6 * 512])
    ident_d = din("ident", [128, 128])
    outT = nc.dram_tensor("outT", [D, SEQ], F32, kind="ExternalOutput").ap()

    HT = [dscr("HT%d" % l, [8, 128, NT], BF16) for l in range(NL)]
    HT2 = [dscr("HT2_%d" % l, [8, 128, NT], BF16) for l in range(NL)]
    QS = [dscr("QS%d" % l, [16, 96, NT], BF16) for l in range(NL)]
    YS = [dscr("YS%d" % l, [16, 65, NT], BF16) for l in range(NL)]
    X1 = [dscr("X1_%d" % l, [8, 128, NT], F32) for l in range(NL)]
    X2 = [dscr("X2_%d" % l, [8, 128, NT], F32) for l in range(NL)]

    def xsrc(l, c):
        t0, W = CH[c]
        if l == 0:
            if c < 8:
                return xT[:, t0:t0 + W].rearrange("(k p) t -> p k t", p=128)
            return ctxT.rearrange("(k p) t -> p k t", p=128)
        return X2[l - 1][:, :, t0:t0 + W].rearrange("k p t -> p k t")

    def sview(T, c):
        t0, W = CH[c]
        return T[:, :, t0:t0 + W].rearrange("k p t -> p k t")

    def sb(name, shape, dt=F32):
        return nc.alloc_sbuf_tensor(name, list(shape), dt).ap()

    ones_bf = sb("ones_bf", [128, 128], BF16)
    ones_f = sb("ones_f", [128, 128], F32)
    ident = sb("ident_sb", [128, 128], F32)
    epsb = sb("epsb", [128, 1], F32)
    cc_t = sb("cc_t", [128, 16], F32)
    sc_t = sb("sc_t", [128, 16], F32)
    modv = sb("modv", [128, 96], F32)
    bm_t = sb("bm_t", [128, 48], F32)
    g1_t = sb("g1_t", [128, 8], F32)
    g2_t = sb("g2_t", [128, 8], F32)
    gf_t = sb("gf_t", [128, 8], F32)
    A1 = sb("A1", [128, 16], F32)
    A2 = sb("A2", [128, 16], F32)
    gqk_t = sb("gqk_t", [64, 4], F32)
    gmq_t = sb("gmq_t", [128, 2], F32)
    gmkv_t = sb("gmkv_t", [128, 1], F32)
    sink_t = sb("sink_t", [1, 6], F32)
    rw_t = sb("rw_t", [128, 128], F32)
    rb_t = sb("rb_t", [128, 16], F32)
    Gs = sb("Gs", [128, NKB * 16], F32)
    rt = [sb("rt%d" % i, [128, 160], F32) for i in range(2)]

    psbig = nc.alloc_psum_tensor("psbig", [128, 4096], F32).ap()
    ps = [psbig[:, i * 512:(i + 1) * 512] for i in range(8)]
    PSK = [("ps", i) for i in range(8)]

    A = Arena(nc, ARENA_WORDS)

    modv3 = modv.rearrange("p (j c) -> p j c", c=2)
    A13 = A1.rearrange("p (j c) -> p j c", c=2)
    A23 = A2.rearrange("p (j c) -> p j c", c=2)
    sc3 = sc_t.rearrange("p (j c) -> p j c", c=2)
    rw3 = rw_t.rearrange("p (k e) -> p k e", e=16)
    Gs3 = Gs.rearrange("p (t e) -> p t e", e=16)

    k.memset("pool", ones_bf, 1.0, writes=["ones_bf"])
    k.memset("pool", ones_f, 1.0, writes=["ones_f"])
    k.memset("pool", epsb, EPS, writes=["epsb"])
    k.dma(ident, ident_d, writes=["ident"])
    k.dma(cc_t, cc, writes=["cc_t"])
    k.dma(gf_t, gf, writes=["gf_t"])
    k.dma(rw_t, rw, writes=["rw_t"])
    k.dma(rb_t, rb.partition_broadcast(128), writes=["rb_t"])
    k.act(sc_t, cc_t, AF.Silu, reads=["cc_t"], writes=["sc_t"])


    def phase_barrier(bump=True):
        P.barrier(bump)

    def norm_mod(xc, W, Acol, Scol, sq, lnv, rstd, tmp, out, out_is_f32, kx, kout, pz, pzk, scale_in=1.0 / D):
        k.act(sq[:, :, :W], xc[:, :, :W], AF.Square, reads=[kx], writes=["nm_sq"])
        for kc in range(8):
            k.mm(pz[:, :W], ones_bf, sq[:, kc, :W], start=(kc == 0), stop=(kc == 7),
                 reads=["nm_sq", "ones_bf"], writes=[pzk])
        k.act(lnv[:, :W], pz[:, :W], AF.Ln, bias=epsb[:, 0:1], scale=scale_in, reads=[pzk, "epsb"], writes=["nm_ln"])
        k.act(rstd[:, :W], lnv[:, :W], AF.Exp, scale=-0.5, reads=["nm_ln"], writes=["nm_rstd"])
        for kc in range(8):
            k.stt(tmp[:, kc, :W], xc[:, kc, :W], Acol[:, kc:kc + 1], rstd[:, :W], ALU.mult, ALU.mult,
                  reads=[kx, "nm_rstd", "modc"], writes=[("nm_tmp", kc)])
            k.act(out[:, kc, :W], tmp[:, kc, :W], AF.Identity, bias=Scol[:, kc:kc + 1], scale=1.0,
                  reads=[("nm_tmp", kc), "modc"], writes=[kout])

    stg_ctr = [0]

    def load_cast(stg, dst, src, key, np_=128, eng="act"):
        Kk, cols = dst.shape[1], dst.shape[2]
        cb = max(1, int(stg[0].shape[1]) // Kk)
        for c0 in range(0, cols, cb):
            c1 = min(cols, c0 + cb)
            i = stg_ctr[0] % len(stg)
            stg_ctr[0] += 1
            sv = stg[i][0:np_, 0:Kk * (c1 - c0)].rearrange("p (k c) -> p k c", k=Kk)
            srcv = src[:, c0:c1].rearrange("(k p) c -> p k c", p=np_)
            k.dma(sv, srcv, writes=[("stg", i)])
            if eng == "act":
                k.act(dst[0:np_, :, c0:c1], sv, AF.Copy, reads=[("stg", i)], writes=[key])
            else:
                k.copy(eng, dst[0:np_, :, c0:c1], sv, reads=[("stg", i)], writes=[key])

    for l in range(NL):
        last = (l == NL - 1)
        phase_barrier()
        A.reset()
        k.dma(bm_t, bm[l], writes=["bm_t"])
        k.dma(g1_t, g1[l], writes=["g1_t"])
        k.dma(g2_t, g2[l], writes=["g2_t"])
        k.dma(gqk_t, gqk[l], writes=["gqk_t"])
        k.dma(gmq_t, gmq[l], writes=["gmq_t"])
        k.dma(gmkv_t, gmkv[l], writes=["gmkv_t"])
        k.dma(sink_t, sink[l], writes=["sink_t"])
        k.act(sink_t, sink_t, AF.Exp, reads=["sink_t"], writes=["sink_t"])
        wst = [A.tile(F32, [8, 512]) for _ in range(2)]
        for j in range(12):
            wv = wst[j % 2]
            k.dma(wv, w_mod[l][:, j * 512:(j + 1) * 512].rearrange("(k p) c -> p k c", p=128), writes=[("wst", j % 2)])
            for jj in range(4):
                f = j * 4 + jj
                for kc in range(8):
                    k.mm(ps[7][:, 2 * f:2 * f + 2], wv[:, kc, jj * 128:(jj + 1) * 128], sc3[:, kc, :],
                         start=(kc == 0), stop=(kc == 7), reads=[("wst", j % 2), "sc_t"], writes=[PSK[7]])
        k.tt("dve", modv3, ps[7][:, 0:96].rearrange("p (j c) -> p j c", c=2),
             bm_t.unsqueeze(2).broadcast_to([128, 48, 2]), ALU.add, reads=[PSK[7], "bm_t"], writes=["modc"])
        k.stt(A13, modv3[:, 8:16, :], 1.0, g1_t.unsqueeze(2).broadcast_to([128, 8, 2]), ALU.add, ALU.mult,
              reads=["modc", "g1_t"], writes=["modc"])
        k.stt(A23, modv3[:, 32:40, :], 1.0, g2_t.unsqueeze(2).broadcast_to([128, 8, 2]), ALU.add, ALU.mult,
              reads=["modc", "g2_t"], writes=["modc"])

        phase_barrier(False)
        A.reset()
        xb = [A.tile(F32, [8, 512]) for _ in range(2)]
        hb = [A.tile(BF16, [8, 512]) for _ in range(2)]
        sq = A.tile(BF16, [8, 512])
        tmp = A.tile(F32, [8, 512])
        lnv = A.tile(F32, [512])
        rstd = A.tile(F32, [512])
        chunks = list(range(9))
        for c in chunks:
            t0, W = CH[c]
            col = 1 if c == 8 else 0
            xc = xb[c % 2]
            k.dma(xc[:, :, :W], xsrc(l, c), writes=[("xb", c % 2)])
            norm_mod(xc, W, A13[:, :, col], modv3[:, 0:8, col], sq, lnv, rstd, tmp, hb[c % 2], False,
                     ("xb", c % 2), ("hb", c % 2), ps[6], PSK[6])
            k.dma(sview(HT[l], c), hb[c % 2][:, :, :W], reads=[("hb", c % 2)], writes=[("HT", c)])

        def rope_out(M, r0, W, a_ps, b_ps, ak, bk, Ct, St, tk, out_ap, wkey, t1, t2, gain=None, rr=None, t3=None, sx=0):
            k1, k2, k3, kr = ("t1", sx), ("t2", sx), ("t3", sx), ("rr", sx)
            if gain is None:
                k.tt("dve", t1[r0:M, :W], a_ps[r0:M, :W], Ct[r0:M, :W], ALU.mult, reads=[ak, tk], writes=[k1])
                k.tt("dve", t2[r0:M, :W], b_ps[r0:M, :W], St[r0:M, :W], ALU.mult, reads=[bk, tk], writes=[k2])
                k.tt("pool", out_ap, t1[r0:M, :W], t2[r0:M, :W], ALU.add, reads=[k1, k2, "KTz"], writes=[wkey])
            else:
                k.stt(t1[r0:M, :W], a_ps[r0:M, :W], gain[0], Ct[r0:M, :W], ALU.mult, ALU.mult,
                      reads=[ak, tk, "gqk_t"], writes=[k1])
                k.stt(t2[r0:M, :W], b_ps[r0:M, :W], gain[1], St[r0:M, :W], ALU.mult, ALU.mult,
                      reads=[bk, tk, "gqk_t"], writes=[k2])
                k.tt("pool", t3[r0:M, :W], t1[r0:M, :W], t2[r0:M, :W], ALU.add, reads=[k1, k2], writes=[k3])
                k.tt("pool", out_ap, t3[r0:M, :W], rr[r0:M, :W], ALU.mult, reads=[k3, kr, "KTz"], writes=[wkey])

        pend = [None]
        actr = [0]

        def flush_pend():
            if pend[0] is not None:
                fn_ = pend[0]
                pend[0] = None
                fn_()

        def attention(hg, dk, KTh, Vh, scale, typ, sink_idx, qb, ptb, den, rdenb, bcs, yst, msk, otsb):
            qchunks = list(range(8)) + ([8] if not last else [])
            ot, otk = ps[6], PSK[6]
            for c in qchunks:
                it = actr[0]
                actr[0] += 1
                t0, W = CH[c]
                qt = qb[it % 2]
                k.dma(qt[0:dk, :W], QS[l][hg, 0:dk, t0:t0 + W], reads=[("QS", hg, c)], writes=[("qb", it % 2)])
                if c == 8:
                    kbs = [(32, None), (33, None)]
                elif typ == "swa":
                    kbs = [(32, None), (33, None)] + [(j, j - 4 * c) for j in range(4 * c - 1, 4 * c + 5) if 0 <= j < 32]
                else:
                    kbs = [(j, None) for j in range(NKB)]
                groups = [kbs[i:i + 2] for i in range(0, len(kbs), 2)]

                def qk_group(gi):
                    b = (gi % 3) * 2
                    for j, (kb, rel) in enumerate(groups[gi]):
                        k.mm(ps[b + j][:, :W], KTh[:, kb * 128:(kb + 1) * 128], qt[:, :W],
                             reads=[("qb", it % 2)], writes=[PSK[b + j]])

                qk_group(0)
                if len(groups) > 1:
                    qk_group(1)
                n = 0
                for gi, g in enumerate(groups):
                    if gi + 2 < len(groups):
                        qk_group(gi + 2)
                    if gi == 1:
                        flush_pend()
                    b = (gi % 3) * 2
                    ng = len(g)
                    pt = ptb[gi % 3]
                    src = psbig[:, b * 512:(b + ng) * 512].rearrange("p (a t) -> p a t", a=ng)[:, :, :W]
                    k.act(pt[:, 0:ng, :W], src, AF.Exp, scale=scale, reads=[PSK[b + j] for j in range(ng)],
                          writes=[("pt", gi % 3)])
                    for j, (kb, rel) in enumerate(g):
                        if rel is not None:
                            k.tt("pool" if n % 2 == 0 else "dve", pt[:, j, :W], pt[:, j, :W], msk[:, rel + 1, :W], ALU.mult,
                                 reads=[("pt", gi % 3), "msk"], writes=[("pt", gi % 3)])
                        k.mm(ot[0:65, :W], Vh[:, kb, :], pt[:, j, :W], start=(n == 0), stop=(n == len(kbs) - 1),
                             reads=[("pt", gi % 3)], writes=[otk])
                        n += 1
                flush_pend()
                osb = otsb[it % 2]
                rd = rdenb[it % 2]
                k.copy("dve", osb[0:65, :W], ot[0:65, :W], reads=[otk], writes=[("otsb", it % 2)])
                if sink_idx is not None:
                    k.ts("dve", den[0:1, :W], osb[0:1, :W], sink_t[0:1, sink_idx:sink_idx + 1], ALU.add,
                         reads=[("otsb", it % 2), "sink_t"], writes=["den"])
                    k.recip(rd[0:1, :W], den[0:1, :W], reads=["den"], writes=[("rden", it % 2)])
                else:
                    k.recip(rd[0:1, :W], osb[0:1, :W], reads=[("otsb", it % 2)], writes=[("rden", it % 2)])

                def e2(osb=osb, rd=rd, it=it, hg=hg, t0=t0, W=W, c=c):
                    k.mm(ps[7][0:65, :W], ones_f[0:1, 0:65], rd[0:1, :W], reads=[("rden", it % 2), "ones_f"], writes=[PSK[7]])
                    k.act(bcs[0:65, :W], ps[7][0:65, :W], AF.Copy, reads=[PSK[7]], writes=["bcs"])
                    ys = yst[it % 2]
                    k.tt("dve", ys[0:65, :W], osb[0:65, :W], bcs[0:65, :W], ALU.mult, reads=[("otsb", it % 2), "bcs"],
                         writes=[("yst", it % 2)])
                    k.dma(YS[l][hg, :, t0:t0 + W], ys[0:65, :W], reads=[("yst", it % 2)], writes=[("YS", c)])

                pend[0] = e2

        for typ in ("swa", "glb", "mla"):
            phase_barrier()
            A.reset()
            nkv = 4 if typ == "mla" else 2
            dk = 96 if typ == "mla" else 64
            KT = A.tile(BF16, [nkv, NT])
            V = A.tile(BF16, [nkv, NKB, 65])
            mark = A.off
            stg = [A.tile(F32, [4096]) for _ in range(2)]
            hb = [A.tile(BF16, [8, 512]) for _ in range(2)]
            tb = [A.tile(F32, [2, 512]) for _ in range(2)]
            t1s = [A.tile(F32, [512]) for _ in range(2)]
            t2s = [A.tile(F32, [512]) for _ in range(2)]
            t3s = [A.tile(F32, [512]) for _ in range(2)]
            rrs = [A.tile(F32, [512]) for _ in range(2)]
            lnvs = [A.tile(F32, [512]) for _ in range(2)]
            sqhs = [A.tile(BF16, [2, 512]) for _ in range(2)]
            t1, t2, t3, rr, lnv, sqh = t1s[0], t2s[0], t3s[0], rrs[0], lnvs[0], sqhs[0]
            qst = [A.tile(BF16, [512]) for _ in range(2)]
            k.memset("pool", V[:, :, :, 0:1], 1.0, writes=["Vones"])
            k.memset("pool", KT, 0.0, writes=["KTz"])
            if typ != "mla":
                base = 0 if typ == "swa" else 640
                hbase = 0 if typ == "swa" else 6
                wq = A.tile(BF16, [8, 384])
                wqs = A.tile(BF16, [8, 384])
                wk = A.tile(BF16, [8, 128])
                wks = A.tile(BF16, [8, 128])
                wv = A.tile(BF16, [8, 128])
                load_cast(stg, wq, w_in[l][:, base:base + 384], "W")
                load_cast(stg, wqs, w_in_sw[l][:, base:base + 384], "W")
                load_cast(stg, wk, w_in[l][:, base + 384:base + 512], "W")
                load_cast(stg, wks, w_in_sw[l][:, base + 384:base + 512], "W")
                load_cast(stg, wv, w_in[l][:, base + 512:base + 640], "W")
                tix = 0
                for c in range(9):
                    t0, W = CH[c]
                    hc = hb[c % 2]
                    k.dma(hc[:, :, :W], sview(HT[l], c), reads=[("HT", c)], writes=[("hb", c % 2)])
                    tc_ = tb[c % 2]
                    k.dma(tc_[0:64, :, :W], tabs[0:2, 0:64, t0:t0 + W].rearrange("a p t -> p a t"), writes=[("tb", c % 2)])
                    Ct, St = tc_[:, 0, :], tc_[:, 1, :]
                    jobs = [("k", kv) for kv in range(2)]
                    if not (last and c == 8):
                        jobs += [("q", h) for h in range(6)]
                    for kind, idx in jobs:
                        sx = tix % 2
                        t1, t2, t3, rr, lnv, sqh = t1s[sx], t2s[sx], t3s[sx], rrs[sx], lnvs[sx], sqhs[sx]
                        wa, wb = (wk, wks) if kind == "k" else (wq, wqs)
                        pa, pb = ps[(tix % 2) * 2], ps[(tix % 2) * 2 + 1]
                        pak, pbk = PSK[(tix % 2) * 2], PSK[(tix % 2) * 2 + 1]
                        for kc in range(8):
                            k.mm(pa[0:64, :W], wa[:, kc, idx * 64:(idx + 1) * 64], hc[:, kc, :W], start=(kc == 0),
                                 stop=(kc == 7), reads=["W", ("hb", c % 2)], writes=[pak])
                        for kc in range(8):
                            k.mm(pb[0:64, :W], wb[:, kc, idx * 64:(idx + 1) * 64], hc[:, kc, :W], start=(kc == 0),
                                 stop=(kc == 7), reads=["W", ("hb", c % 2)], writes=[pbk])
                        if kind == "k":
                            out_ap, wkey = KT[0:64, idx, t0:t0 + W], ("KT", idx, c)
                        else:
                            out_ap, wkey = qst[tix % 2][0:64, :W], ("qst", tix % 2)
                        if typ == "glb":
                            go = 0 if kind == "q" else 2
                            k.act(sqh[0:64, 0, :W], pa[0:64, :W], AF.Square, reads=[pak], writes=[("sqh", sx)])
                            k.mm(ps[6][0:64, :W], ones_bf[0:64, 0:64], sqh[0:64, 0, :W], reads=[("sqh", sx), "ones_bf"], writes=[PSK[6]])
                            k.act(lnv[0:64, :W], ps[6][0:64, :W], AF.Ln, bias=epsb[0:64, 0:1], scale=1.0 / 64,
                                  reads=[PSK[6], "epsb"], writes=[("lnv", sx)])
                            k.act(rr[0:64, :W], lnv[0:64, :W], AF.Exp, scale=-0.5, reads=[("lnv", sx)], writes=[("rr", sx)])
                            rope_out(64, 0, W, pa, pb, pak, pbk, Ct, St, ("tb", c % 2), out_ap, wkey, t1, t2,
                                     gain=(gqk_t[:, go:go + 1], gqk_t[:, go + 1:go + 2]), rr=rr, t3=t3, sx=sx)
                        else:
                            rope_out(64, 0, W, pa, pb, pak, pbk, Ct, St, ("tb", c % 2), out_ap, wkey, t1, t2, sx=sx)
                        if kind == "q":
                            k.dma(QS[l][hbase + idx, 0:64, t0:t0 + W], out_ap, reads=[wkey], writes=[("QS", hbase + idx, c)])
                        tix += 1
                    for tt_ in range(W // 128):
                        kb = t0 // 128 + tt_
                        for kc in range(8):
                            k.mm(ps[5][:, 0:128], hc[:, kc, tt_ * 128:(tt_ + 1) * 128], wv[:, kc, :], start=(kc == 0),
                                 stop=(kc == 7), reads=["W", ("hb", c % 2)], writes=[PSK[5]])
                        k.copy("dve", V[:, :, kb, 1:65], ps[5][:, 0:128].rearrange("p (a d) -> p a d", a=2),
                               reads=[PSK[5], "Vones"], writes=[("V", kb)])
            else:
                wql = A.tile(BF16, [8, 256])
                wkvl = A.tile(BF16, [8, 128])
                wkr = A.tile(BF16, [8, 96])
                wkrs = A.tile(BF16, [8, 96])
                wuq = A.tile(BF16, [2, 384])
                wuqs = A.tile(BF16, [2, 384])
                wukv = A.tile(BF16, [1, 512])
                latq = A.tile(BF16, [2, 512])
                latkv = A.tile(BF16, [512])
                k.memset("pool", wkr, 0.0, writes=["W"])
                k.memset("pool", wkrs, 0.0, writes=["W"])
                load_cast(stg, wql, w_in[l][:, 1280:1536], "W")
                load_cast(stg, wkvl, w_in[l][:, 1536:1664], "W")
                load_cast(stg, wkr[:, :, 64:96], w_in[l][:, 1664:1696], "W")
                load_cast(stg, wkrs[:, :, 64:96], w_in_sw[l][:, 1664:1696], "W")
                load_cast(stg, wuq, w_uq[l], "W")
                load_cast(stg, wuqs, w_uq_sw[l], "W")
                load_cast(stg, wukv, w_ukv[l], "W")
                tix = 0
                for c in range(9):
                    t0, W = CH[c]
                    hc = hb[c % 2]
                    k.dma(hc[:, :, :W], sview(HT[l], c), reads=[("HT", c)], writes=[("hb", c % 2)])
                    tc_ = tb[c % 2]
                    k.dma(tc_[0:96, :, :W], tabs[2:4, 0:96, t0:t0 + W].rearrange("a p t -> p a t"), writes=[("tb", c % 2)])
                    Ct, St = tc_[:, 0, :], tc_[:, 1, :]
                    needq = not (last and c == 8)
                    if needq:
                        for kc2 in range(2):
                            for kc in range(8):
                                k.mm(ps[kc2][:, :W], wql[:, kc, kc2 * 128:(kc2 + 1) * 128], hc[:, kc, :W], start=(kc == 0),
                                     stop=(kc == 7), reads=["W", ("hb", c % 2)], writes=[PSK[kc2]])
                            k.act(sqh[:, kc2, :W], ps[kc2][:, :W], AF.Square, reads=[PSK[kc2]], writes=[("sqh", kc2)])
                        for kc2 in range(2):
                            k.mm(ps[6][:, :W], ones_bf, sqh[:, kc2, :W], start=(kc2 == 0), stop=(kc2 == 1),
                                 reads=[("sqh", kc2), "ones_bf"], writes=[PSK[6]])
                        k.act(lnv[:, :W], ps[6][:, :W], AF.Ln, bias=epsb[:, 0:1], scale=1.0 / 256, reads=[PSK[6], "epsb"], writes=["lnv"])
                        k.act(rr[:, :W], lnv[:, :W], AF.Exp, scale=-0.5, reads=["lnv"], writes=["rr"])
                        for kc2 in range(2):
                            k.stt(latq[:, kc2, :W], ps[kc2][:, :W], gmq_t[:, kc2:kc2 + 1], rr[:, :W], ALU.mult, ALU.mult,
                                  reads=[PSK[kc2], "rr", "gmq_t"], writes=["latq"])
                    for kc in range(8):
                        k.mm(ps[2][:, :W], wkvl[:, kc, :], hc[:, kc, :W], start=(kc == 0), stop=(kc == 7),
                             reads=["W", ("hb", c % 2)], writes=[PSK[2]])
                    k.act(sqh[:, 0, :W], ps[2][:, :W], AF.Square, reads=[PSK[2]], writes=[("sqh", 0)])
                    k.mm(ps[6][:, :W], ones_bf, sqh[:, 0, :W], reads=[("sqh", 0), "ones_bf"], writes=[PSK[6]])
                    k.act(lnv[:, :W], ps[6][:, :W], AF.Ln, bias=epsb[:, 0:1], scale=1.0 / 128, reads=[PSK[6], "epsb"], writes=["lnv"])
                    k.act(rr[:, :W], lnv[:, :W], AF.Exp, scale=-0.5, reads=["lnv"], writes=["rr"])
                    k.stt(latkv[:, :W], ps[2][:, :W], gmkv_t[:, 0:1], rr[:, :W], ALU.mult, ALU.mult,
                          reads=[PSK[2], "rr", "gmkv_t"], writes=["latkv"])
                    for kc in range(8):
                        k.mm(ps[0][0:96, :W], wkr[:, kc, :], hc[:, kc, :W], start=(kc == 0), stop=(kc == 7),
                             reads=["W", ("hb", c % 2)], writes=[PSK[0]])
                    for kc in range(8):
                        k.mm(ps[1][0:96, :W], wkrs[:, kc, :], hc[:, kc, :W], start=(kc == 0), stop=(kc == 7),
                             reads=["W", ("hb", c % 2)], writes=[PSK[1]])
                    rope_out(96, 64, W, ps[0], ps[1], PSK[0], PSK[1], Ct, St, ("tb", c % 2), KT[64:96, 0, t0:t0 + W], ("KTr", 0, c), t1, t2)
                    for h in range(1, 4):
                        k.copy("pool", KT[64:96, h, t0:t0 + W], KT[64:96, 0, t0:t0 + W], reads=[("KTr", 0, c)], writes=[("KTr", h, c)])
                    for h in range(4):
                        k.mm(ps[3][0:64, :W], wukv[:, 0, h * 128:h * 128 + 64], latkv[:, :W], reads=["W", "latkv"], writes=[PSK[3]])
                        k.act(KT[0:64, h, t0:t0 + W], ps[3][0:64, :W], AF.Copy, reads=[PSK[3], "KTz"], writes=[("KTn", h, c)])
                        if needq:
                            pa, pb = ps[(tix % 2) * 2], ps[(tix % 2) * 2 + 1]
                            pak, pbk = PSK[(tix % 2) * 2], PSK[(tix % 2) * 2 + 1]
                            for kc2 in range(2):
                                k.mm(pa[0:96, :W], wuq[:, kc2, h * 96:(h + 1) * 96], latq[:, kc2, :W], start=(kc2 == 0),
                                     stop=(kc2 == 1), reads=["W", "latq"], writes=[pak])
                            for kc2 in range(2):
                                k.mm(pb[0:96, :W], wuqs[:, kc2, h * 96:(h + 1) * 96], latq[:, kc2, :W], start=(kc2 == 0),
                                     stop=(kc2 == 1), reads=["W", "latq"], writes=[pbk])
                            out_ap, wkey = qst[tix % 2][0:96, :W], ("qst", tix % 2)
                            rope_out(96, 0, W, pa, pb, pak, pbk, Ct, St, ("tb", c % 2), out_ap, wkey, t1, t2)
                            k.dma(QS[l][12 + h, 0:96, t0:t0 + W], out_ap, reads=[wkey], writes=[("QS", 12 + h, c)])
                            tix += 1
                    wv4 = wukv[:, 0, :].rearrange("p (h d) -> p h d", h=4)[:, :, 64:128]
                    for tt_ in range(W // 128):
                        kb = t0 // 128 + tt_
                        k.mm(ps[5][:, 0:256].rearrange("p (h d) -> p h d", h=4), latkv[:, tt_ * 128:(tt_ + 1) * 128], wv4,
                             reads=["W", "latkv"], writes=[PSK[5]])
                        k.copy("dve", V[:, :, kb, 1:65], ps[5][:, 0:256].rearrange("p (a d) -> p a d", a=4),
                               reads=[PSK[5], "Vones"], writes=[("V", kb)])
            phase_barrier(typ == "glb")
            A.reset(mark)
            qb = [A.tile(BF16, [512]) for _ in range(2)]
            for i_ in range(2):
                k.memset("pool", qb[i_], 0.0, writes=[("qb", i_)])
            ptb = [A.tile(BF16, [2, 512]) for _ in range(3)]
            otsb = [A.tile(F32, [512]) for _ in range(2)]
            rdenb = [A.tile(F32, [512]) for _ in range(2)]
            yst = [A.tile(BF16, [512]) for _ in range(2)]
            den = A.tile(F32, [512])
            rden = A.tile(F32, [512])
            bcs = A.tile(F32, [512])
            msk = A.tile(BF16, [6, 512])
            if typ == "swa":
                mst = A.tile(F32, [6, 512])
                k.dma(mst, masks.rearrange("p (a t) -> p a t", a=6), writes=["mst"])
                k.copy("pool", msk, mst, reads=["mst"], writes=["msk"])
            nh = 4 if typ == "mla" else 6
            hbase = {"swa": 0, "glb": 6, "mla": 12}[typ]
            for h in range(nh):
                kvh = h if typ == "mla" else h // 3
                scale = (96 ** -0.5) if typ == "mla" else 0.125
                attention(hbase + h, dk, KT[:, kvh, :], V[:, kvh, :, :], scale, typ, h if typ == "swa" else None,
                          qb, ptb, den, rdenb, bcs, yst, msk, otsb)
            flush_pend()

        phase_barrier()
        A.reset()
        nch = 8 if last else 9
        stg = [A.tile(F32, [4096]) for _ in range(1)]
        wo = A.tile(BF16, [16, 1024])
        k.memset("pool", wo, 0.0, writes=["wo"])
        for hh in range(0, 16, 4):
            sv = stg[0][:, 0:4096].rearrange("p (h c) -> p h c", h=4)
            k.memset("pool", sv[0:1, :, :], 0.0, writes=[("stg", 0)])
            k.dma(sv[1:65, :, :], w_out[l][hh * 64:(hh + 4) * 64, :].rearrange("(h r) c -> r h c", r=64), writes=[("stg", 0)])
            k.act(wo[0:65, hh:hh + 4, :], sv[0:65, :, :], AF.Copy, reads=[("stg", 0)], writes=["wo"])
        ytb = [A.tile(BF16, [16, 512]) for _ in range(1)]
        k.memset("pool", ytb[0], 0.0, writes=[("ytb", 0)])
        xb = [A.tile(F32, [8, 512]) for _ in range(2)]
        x1bs = [A.tile(F32, [8, 512]) for _ in range(2)]
        h32 = A.tile(F32, [8, 512])
        h2b = [A.tile(BF16, [8, 512]) for _ in range(2)]
        sq = A.tile(BF16, [8, 512])
        tmp = A.tile(F32, [8, 512])
        lnv = A.tile(F32, [512])
        rstd = A.tile(F32, [512])
        tile_i = 0
        for c in range(nch):
            t0, W = CH[c]
            col = 1 if c == 8 else 0
            yt = ytb[0]
            x1b = x1bs[c % 2]
            x1k = ("x1b", c % 2)
            k.dma(yt[0:65, :, :W], YS[l][:, :, t0:t0 + W].rearrange("h r t -> r h t"), reads=[("YS", c)], writes=[("ytb", 0)])
            xc = xb[c % 2]
            k.dma(xc[:, :, :W], xsrc(l, c), writes=[("xb", c % 2)])
            for dc in range(8):
                po, pok = ps[dc % 2], PSK[dc % 2]
                for hh in range(16):
                    k.mm(po[:, :W], wo[:, hh, dc * 128:(dc + 1) * 128], yt[:, hh, :W], start=(hh == 0), stop=(hh == 15),
                         reads=["wo", ("ytb", 0)], writes=[pok])
                k.stt(x1b[:, dc, :W], po[:, :W], modv3[:, 16 + dc, col:col + 1], xc[:, dc, :W], ALU.mult, ALU.add,
                      reads=[pok, ("xb", c % 2), "modc"], writes=[x1k])
            k.dma(sview(X1[l], c), x1b[:, :, :W], reads=[x1k], writes=[("X1", c)])
            norm_mod(x1b, W, A23[:, :, col], modv3[:, 24:32, col], sq, lnv, rstd, tmp, h32, True,
                     x1k, "h32", ps[6], PSK[6])
            h2 = h2b[c % 2]
            k.copy("pool", h2[:, :, :W], h32[:, :, :W], reads=["h32"], writes=[("h2b", c % 2)])
            k.dma(sview(HT2[l], c), h2[:, :, :W], reads=[("h2b", c % 2)], writes=[("HT2", c)])
            for tt_ in range(W // 128):
                R = rt[tile_i % 2]
                rk = ("rt", tile_i % 2)
                lg = ps[4 + tile_i % 2][:, 0:16]
                lgk = PSK[4 + tile_i % 2]
                for kc in range(8):
                    k.mm(lg, h32[:, kc, tt_ * 128:(tt_ + 1) * 128], rw3[:, kc, :], start=(kc == 0), stop=(kc == 7),
                         reads=["h32", "rw_t"], writes=[lgk])
                e1, scv, bs, eq, b2, ge, selv, gu, G = [R[:, i * 16:(i + 1) * 16] for i in range(9)]
                m1, m2, gs, gsel = [R[:, 144 + i * 4:148 + i * 4] for i in range(4)]
                v3 = lambda ap: ap.rearrange("p (g e) -> p g e", e=4)
                bc4 = lambda ap: ap.unsqueeze(2).broadcast_to([128, 4, 4])
                k.act(e1, lg, AF.Exp, scale=-1.0, reads=[lgk], writes=[rk])
                k.ts("dve", e1, e1, 1.0, ALU.add, reads=[rk], writes=[rk])
                k.recip(scv, e1, reads=[rk], writes=[rk])
                k.tt("dve", bs, scv, rb_t, ALU.add, reads=[rk, "rb_t"], writes=[rk])
                k.reduce(m1, v3(bs), ALU.max, AX.X, reads=[rk], writes=[rk])
                k.tt("dve", v3(eq), v3(bs), bc4(m1), ALU.is_equal, reads=[rk], writes=[rk])
                k.stt(v3(b2), v3(eq), -1e9, v3(bs), ALU.mult, ALU.add, reads=[rk], writes=[rk])
                k.reduce(m2, v3(b2), ALU.max, AX.X, reads=[rk], writes=[rk])
                k.tt("dve", gs, m1, m2, ALU.add, reads=[rk], writes=[rk])
                k.reduce(e1[:, 0:1], gs, ALU.max, AX.X, reads=[rk], writes=[rk])
                k.ts("dve", gsel, gs, e1[:, 0:1], ALU.is_equal, reads=[rk], writes=[rk])
                k.tt("dve", v3(ge), v3(bs), bc4(m2), ALU.is_ge, reads=[rk], writes=[rk])
                k.tt("dve", v3(selv), v3(ge), bc4(gsel), ALU.mult, reads=[rk], writes=[rk])
                k.tt("dve", gu, scv, selv, ALU.mult, reads=[rk], writes=[rk])
                k.reduce(e1[:, 1:2], gu, ALU.add, AX.X, reads=[rk], writes=[rk])
                k.recip(e1[:, 2:3], e1[:, 1:2], reads=[rk], writes=[rk])
                tok_tile = t0 // 128 + tt_
                k.ts("dve", Gs3[:, tok_tile, :], gu, e1[:, 2:3], ALU.mult, reads=[rk], writes=[("Gs", tok_tile)])
                tile_i += 1

        phase_barrier()
        A.reset()
        scs = [[0, 1, 2], [3, 4, 5], [6, 7] if last else [6, 7, 8]]
        stg = [A.tile(F32, [2048]) for _ in range(3)]
        diag = A.tile(F32, [4, 128])
        wgb = [A.tile(BF16, [8, 512]) for _ in range(2)]
        wub = [A.tile(BF16, [8, 512]) for _ in range(2)]
        wdb = [A.tile(BF16, [4, 1024]) for _ in range(2)]
        h2b = [A.tile(BF16, [8, 512]) for _ in range(2)]
        actb = [A.tile(BF16, [4, 512]) for _ in range(2)]
        acc = A.tile(F32, [8, 1536])
        bcs = A.tile(F32, [512])
        sgb = [A.tile(F32, [512]) for _ in range(2)]
        ugb = [A.tile(BF16, [512]) for _ in range(2)]
        x1c = A.tile(F32, [8, 512])
        sq = A.tile(BF16, [8, 512])
        lnv = A.tile(F32, [512])
        rstd = A.tile(F32, [512])
        items = [(si, e) for si in range(len(scs)) for e in range(17)]
        steps = []
        for n_, (si, e) in enumerate(items):
            o = 0
            for sidx, c in enumerate(scs[si]):
                steps.append(dict(n=n_, si=si, e=e, c=c, sidx=sidx, nsteps=len(scs[si]), k=len(steps), off=o))
                o += CH[c][1]

        def piece(n_, p):
            si, e = items[n_]
            b = n_ % 2
            srcs = (ewg[l, e], ewu[l, e], ewd[l, e]) if e < 16 else (swg[l], swu[l], swd[l])
            if p < 2:
                dst = wgb[b][:, :, p * 256:(p + 1) * 256]
                src = srcs[0][:, p * 256:(p + 1) * 256].rearrange("(k p) c -> p k c", p=128)
                Kk = 8
            elif p < 4:
                q = p - 2
                dst = wub[b][:, :, q * 256:(q + 1) * 256]
                src = srcs[1][:, q * 256:(q + 1) * 256].rearrange("(k p) c -> p k c", p=128)
                Kk = 8
            else:
                q = p - 4
                dst = wdb[b][:, :, q * 512:(q + 1) * 512]
                src = srcs[2][:, q * 512:(q + 1) * 512].rearrange("(k p) c -> p k c", p=128)
                Kk = 4
            sv = stg[p % 3].rearrange("p (k c) -> p k c", k=Kk)
            return dst, src, sv, ("wexp", b, p)

        def piece_dma(n_, p):
            dst, src, sv, key = piece(n_, p)
            k.dma(sv, src, writes=[("stg", p % 3)])

        def piece_cast(n_, p):
            dst, src, sv, key = piece(n_, p)
            k.act(dst, sv, AF.Copy, reads=[("stg", p % 3)], writes=[key])

        def h2_dma(st):
            t0, W = CH[st["c"]]
            kk = st["k"]
            k.dma(h2b[kk % 2][:, :, :W], sview(HT2[l], st["c"]), reads=[("HT2", st["c"])], writes=[("h2b", kk % 2)])

        def gu_part(st):
            n_, e, c, sidx, nsteps, kk = st["n"], st["e"], st["c"], st["sidx"], st["nsteps"], st["k"]
            t0, W = CH[c]
            b = n_ % 2
            wg, wu = wgb[b], wub[b]
            pre = n_ + 1 < len(items)
            if pre:
                if nsteps == 3:
                    if sidx == 0:
                        for p in (0, 1, 2):
                            piece_dma(n_ + 1, p)
                    elif sidx == 1:
                        for p in (0, 1, 2):
                            piece_cast(n_ + 1, p)
                        for p in (3, 4, 5):
                            piece_dma(n_ + 1, p)
                    else:
                        for p in (3, 4, 5):
                            piece_cast(n_ + 1, p)
                else:
                    for p in ((0, 1, 2) if sidx == 0 else (3, 4, 5)):
                        piece_dma(n_ + 1, p)
            if kk + 1 < len(steps):
                h2_dma(steps[kk + 1])
            h2 = h2b[kk % 2]
            hk = ("h2b", kk % 2)
            if e < 16:
                for tt_ in range(W // 128):
                    tok_tile = t0 // 128 + tt_
                    k.ts("dve", diag[:, tt_, :], ident, Gs3[:, tok_tile, e:e + 1], ALU.mult,
                         reads=["ident"], writes=[("diag", tt_)])
                    k.mm(ps[6][:, tt_ * 128:(tt_ + 1) * 128], ones_f, diag[:, tt_, :],
                         reads=[("diag", tt_), "ones_f"], writes=[PSK[6]])
                k.act(bcs[:, :W], ps[6][:, :W], AF.Copy, reads=[PSK[6]], writes=["bcs"])
            act_t = actb[kk % 2]
            ak = ("act", kk % 2)
            for fc in range(4):
                pg, pgk = ps[fc % 2], PSK[fc % 2]
                pu, puk = ps[2 + fc % 2], PSK[2 + fc % 2]
                wgk = ("wexp", b, 0 if fc < 2 else 1)
                wuk = ("wexp", b, 2 if fc < 2 else 3)
                for kc in range(8):
                    k.mm(pg[:, :W], wg[:, kc, fc * 128:(fc + 1) * 128], h2[:, kc, :W], start=(kc == 0), stop=(kc == 7),
                         reads=[wgk, hk], writes=[pgk])
                for kc in range(8):
                    k.mm(pu[:, :W], wu[:, kc, fc * 128:(fc + 1) * 128], h2[:, kc, :W], start=(kc == 0), stop=(kc == 7),
                         reads=[wuk, hk], writes=[puk])
                sg, ug = sgb[fc % 2], ugb[fc % 2]
                k.act(sg[:, :W], pg[:, :W], AF.Silu, reads=[pgk], writes=[("sg", fc % 2)])
                if e < 16:
                    k.tt("dve", ug[:, :W], pu[:, :W], bcs[:, :W], ALU.mult, reads=[puk, "bcs"], writes=[("ug", fc % 2)])
                else:
                    k.copy("dve", ug[:, :W], pu[:, :W], reads=[puk], writes=[("ug", fc % 2)])
                k.tt("pool", act_t[:, fc, :W], sg[:, :W], ug[:, :W], ALU.mult,
                     reads=[("sg", fc % 2), ("ug", fc % 2)], writes=[ak])
            if pre and nsteps != 3:
                for p in ((0, 1, 2) if sidx == 0 else (3, 4, 5)):
                    piece_cast(n_ + 1, p)

        def down_part(st):
            n_, e, c, sidx, kk, off = st["n"], st["e"], st["c"], st["sidx"], st["k"], st["off"]
            t0, W = CH[c]
            b = n_ % 2
            wd = wdb[b]
            act_t = actb[kk % 2]
            ak = ("act", kk % 2)
            for dc in range(8):
                py, pyk = ps[4 + dc % 2], PSK[4 + dc % 2]
                wdk = ("wexp", b, 4 if dc < 4 else 5)
                for fc in range(4):
                    k.mm(py[:, :W], wd[:, fc, dc * 128:(dc + 1) * 128], act_t[:, fc, :W], start=(fc == 0), stop=(fc == 3),
                         reads=[wdk, ak], writes=[pyk])
                av = acc[:, dc, off:off + W]
                if e == 0:
                    k.copy("dve", av, py[:, :W], reads=[pyk], writes=[("acc", sidx, dc)])
                else:
                    k.tt("dve", av, py[:, :W], av, ALU.add, reads=[pyk, ("acc", sidx, dc)], writes=[("acc", sidx, dc)])

        def epilogue(si):
            o = 0
            for sidx, c in enumerate(scs[si]):
                t0, W = CH[c]
                col = 1 if c == 8 else 0
                off = o
                o += W
                k.dma(x1c[:, :, :W], sview(X1[l], c), reads=[("X1", c)], writes=["x1c"])
                for dc in range(8):
                    k.stt(x1c[:, dc, :W], acc[:, dc, off:off + W], modv3[:, 40 + dc, col:col + 1], x1c[:, dc, :W],
                          ALU.mult, ALU.add, reads=[("acc", sidx, dc), "x1c", "modc"], writes=["x1c"])
                if not last:
                    k.dma(sview(X2[l], c), x1c[:, :, :W], reads=["x1c"], writes=[("X2", c)])
                else:
                    k.act(sq[:, :, :W], x1c[:, :, :W], AF.Square, reads=["x1c"], writes=["fsq"])
                    for kc in range(8):
                        k.mm(ps[7][:, :W], ones_bf, sq[:, kc, :W], start=(kc == 0), stop=(kc == 7), reads=["fsq", "ones_bf"], writes=[PSK[7]])
                    k.act(lnv[:, :W], ps[7][:, :W], AF.Ln, bias=epsb[:, 0:1], scale=1.0 / D, reads=[PSK[7], "epsb"], writes=["flnv"])
                    k.act(rstd[:, :W], lnv[:, :W], AF.Exp, scale=-0.5, reads=["flnv"], writes=["frstd"])
                    for kc in range(8):
                        k.stt(acc[:, kc, off:off + W], x1c[:, kc, :W], gf_t[:, kc:kc + 1], rstd[:, :W], ALU.mult, ALU.mult,
                              reads=["x1c", "frstd", "gf_t", ("acc", sidx, kc)], writes=[("acc", sidx, kc)])
                    k.dma(outT[:, t0:t0 + W].rearrange("(k p) t -> p k t", p=128), acc[:, :, off:off + W],
                          reads=[("acc", sidx, kc) for kc in range(8)], writes=[("out", c)])

        for p in range(6):
            piece_dma(0, p)
            piece_cast(0, p)
        h2_dma(steps[0])
        gu_part(steps[0])
        for kk in range(len(steps)):
            st = steps[kk]
            if kk + 1 < len(steps):
                gu_part(steps[kk + 1])
            down_part(st)
            if st["e"] == 16 and st["sidx"] == st["nsteps"] - 1:
                epilogue(st["si"])

    P.emit()
    return nc


def _rope_tables():
    theta = 10000.0
    t = np.arange(SEQ)
    rows = (t // 64).astype(np.float64)
    cols = (t % 64).astype(np.float64)
    tabs = np.zeros((4, 128, NT), np.float32)
    tabs[0, :, :] = 1.0
    tabs[2, :, :] = 1.0

    def fill(ci, si, r0, nf):
        inv = theta ** (-np.arange(nf, dtype=np.float64) / nf)
        for half, pos in enumerate((rows, cols)):
            ang = pos[None, :] * inv[:, None]
            b = r0 + half * 2 * nf
            tabs[ci, b:b + nf, :SEQ] = np.cos(ang)
            tabs[ci, b + nf:b + 2 * nf, :SEQ] = np.cos(ang)
            tabs[si, b:b + nf, :SEQ] = -np.sin(ang)
            tabs[si, b + nf:b + 2 * nf, :SEQ] = np.sin(ang)

    fill(0, 1, 0, 16)
    fill(2, 3, 64, 8)
    return tabs


def _swap_perm(n, nf):
    p = np.arange(n)
    out = p.copy()
    for b in range(0, n, 2 * nf):
        out[b:b + nf] = p[b + nf:b + 2 * nf]
        out[b + nf:b + 2 * nf] = p[b:b + nf]
    return out


def _masks():
    a = np.arange(128)
    lo = (a[None, :] <= a[:, None]).astype(np.float32)
    hi = (a[None, :] >= a[:, None]).astype(np.float32)
    one = np.ones((128, 128), np.float32)
    zero = np.zeros((128, 128), np.float32)
    m = np.zeros((128, 6, 512), np.float32)
    for r in range(6):
        rel = r - 1
        for i in range(4):
            d = rel - i
            blk = one if d == 0 else lo if d == -1 else hi if d == 1 else zero
            m[:, r, i * 128:(i + 1) * 128] = blk
    return m.reshape(128, 6 * 512)


_NC_CACHE = {}


def kernel(x, c, ctx, c_ctx, w_mod, b_mod, norm_mix_g, norm_ffn_g, w_in, w_out, swa_sink,
           glb_q_gain, glb_k_gain, mla_q_gain, mla_w_uq, mla_kv_gain, mla_w_ukv,
           router_w, router_bias, exp_w_gate, exp_w_up, exp_w_down,
           shr_w_gate, shr_w_up, shr_w_down, final_norm_g, _dbg=False, _cores=None):
    f = lambda a: np.ascontiguousarray(np.asarray(a, dtype=np.float32))
    x, c, ctx, c_ctx = f(x), f(c), f(ctx), f(c_ctx)
    w_in = f(w_in)
    perm = np.arange(1696)
    p64 = _swap_perm(64, 16)
    for hb_ in list(range(0, 640, 64)) + list(range(640, 1280, 64)):
        perm[hb_:hb_ + 64] = hb_ + p64
    perm[1664:1696] = 1664 + _swap_perm(32, 8)
    w_in_sw = np.ascontiguousarray(w_in[:, :, perm])
    w_uq = f(mla_w_uq)
    pu = np.arange(384)
    for h in range(4):
        pu[h * 96 + 64:h * 96 + 96] = h * 96 + 64 + _swap_perm(32, 8)
    w_uq_sw = np.ascontiguousarray(w_uq[:, :, pu])
    gq, gk = f(glb_q_gain), f(glb_k_gain)
    gqk = np.stack([gq, gq[:, p64], gk, gk[:, p64]], axis=-1)
    gmq = np.ascontiguousarray(f(mla_q_gain).reshape(NL, 2, 128).transpose(0, 2, 1))
    gmkv = f(mla_kv_gain).reshape(NL, 128, 1)
    bm = np.ascontiguousarray(f(b_mod).reshape(NL, 48, 128).transpose(0, 2, 1))
    g1 = np.ascontiguousarray(f(norm_mix_g).reshape(NL, 8, 128).transpose(0, 2, 1))
    g2 = np.ascontiguousarray(f(norm_ffn_g).reshape(NL, 8, 128).transpose(0, 2, 1))
    gfin = np.ascontiguousarray(f(final_norm_g).reshape(8, 128).T)
    rw = np.ascontiguousarray(f(router_w).reshape(8, 128, 16).transpose(1, 0, 2).reshape(128, 128))
    shared = {
        "w_mod": f(w_mod), "bm": bm, "g1": g1, "g2": g2, "gf": gfin, "w_in": w_in, "w_in_sw": w_in_sw,
        "w_out": f(w_out), "sink": f(swa_sink).reshape(NL, 1, 6), "gqk": np.ascontiguousarray(gqk), "gmq": gmq, "gmkv": gmkv,
        "w_uq": w_uq, "w_uq_sw": w_uq_sw, "w_ukv": f(mla_w_ukv), "rw": rw, "rb": f(router_bias),
        "ewg": f(exp_w_gate), "ewu": f(exp_w_up), "ewd": f(exp_w_down),
        "swg": f(shr_w_gate), "swu": f(shr_w_up), "swd": f(shr_w_down),
        "tabs": _rope_tables(), "masks": _masks(), "ident": np.eye(128, dtype=np.float32),
    }
    cores = list(range(8)) if _cores is None else _cores
    in_maps = []
    for b in cores:
        m = dict(shared)
        m["xT"] = np.ascontiguousarray(x[b].T)
        m["ctxT"] = np.ascontiguousarray(ctx[b].T)
        ccv = np.stack([c[b], c_ctx], axis=-1).reshape(8, 128, 2).transpose(1, 0, 2).reshape(128, 16)
        m["cc"] = np.ascontiguousarray(ccv)
        in_maps.append(m)
    key = bool(_dbg)
    if key not in _NC_CACHE:
        _NC_CACHE[key] = build_program(dbg=_dbg)
    nc = _NC_CACHE[key]
    res = run_bass_kernel_spmd(nc, in_maps, core_ids=list(range(len(cores))))
    if _dbg:
        return res
    out = np.stack([np.ascontiguousarray(r["outT"].T) for r in res.results], axis=0)
    return out.astype(np.float32)
```

```python
import numpy as np
import concourse.bass as bass
import concourse.mybir as mybir
from concourse.bass_utils import run_bass_kernel_spmd

F32 = mybir.dt.float32
BF16 = mybir.dt.bfloat16
I32 = mybir.dt.int32
U32 = mybir.dt.uint32
ALU = mybir.AluOpType
AF = mybir.ActivationFunctionType

ENGS = ("pe", "dve", "act", "pool", "sp")
NDMASEM = 24


class Op:
    __slots__ = ("eng", "fn", "deps", "needs_inc", "val", "dma", "dsem", "dval", "idx", "epoch")

    def __init__(self, eng, fn, dma=False):
        self.eng = eng
        self.fn = fn
        self.deps = []
        self.needs_inc = False
        self.val = 0
        self.dma = dma
        self.dsem = None
        self.dval = 0
        self.idx = 0
        self.epoch = 0


class Prog:
    def __init__(self, nc):
        self.nc = nc
        self.ops = {e: [] for e in ENGS}
        self.last_w = {}
        self.readers = {}
        self.ndma = 0
        self.dma_prev = [None] * NDMASEM
        self.all_ops = []
        self.epoch = 0

    def _add(self, eng, fn, reads, writes, dma=False):
        op = Op(eng, fn, dma)
        op.idx = len(self.ops[eng])
        op.epoch = self.epoch
        deps = []
        for k in reads:
            w = self.last_w.get(k)
            if w is not None:
                deps.append(w)
            if isinstance(k, tuple) and k and k[0] == "ps":
                for r in self.readers.get(k, ()):
                    if r.eng != eng:
                        deps.append(r)
        for k in writes:
            w = self.last_w.get(k)
            if w is not None:
                deps.append(w)
            for r in self.readers.get(k, ()):
                if r.eng != eng or r.dma or dma:
                    deps.append(r)
        if dma:
            s = self.ndma % NDMASEM
            self.ndma += 1
            prev = self.dma_prev[s]
            op.dsem = s
            op.dval = (prev.dval if prev is not None else 0) + 16
            if prev is not None:
                deps.append(prev)
            self.dma_prev[s] = op
        seen = set()
        for d in deps:
            if d is op or id(d) in seen:
                continue
            seen.add(id(d))
            if (not d.dma) and d.eng == eng and eng == "pe" and not dma:
                continue
            op.deps.append(d)
            if not d.dma:
                d.needs_inc = True
        for k in reads:
            self.readers.setdefault(k, []).append(op)
        for k in writes:
            self.last_w[k] = op
            self.readers[k] = []
        self.ops[eng].append(op)
        self.all_ops.append(op)
        return op

    def pe(self, fn, reads=(), writes=()):
        return self._add("pe", fn, reads, writes)

    def dve(self, fn, reads=(), writes=()):
        return self._add("dve", fn, reads, writes)

    def act(self, fn, reads=(), writes=()):
        return self._add("act", fn, reads, writes)

    def pool(self, fn, reads=(), writes=()):
        return self._add("pool", fn, reads, writes)

    def dma(self, fn, reads=(), writes=(), q="sp"):
        return self._add(q, fn, reads, writes, dma=True)

    def barrier(self, bump=True):
        key = ("__barrier__", len(self.all_ops))
        lasts = []
        for e in ENGS:
            for op in reversed(self.ops[e]):
                if (not op.dma) and op.fn is not None:
                    lasts.append(op)
                    break
        for d in self.dma_prev:
            if d is not None:
                lasts.append(d)
        self._barrier_deps = lasts
        self.pending_barrier = {e: list(lasts) for e in ENGS}
        for e in ENGS:
            op = Op(e, None, False)
            op.idx = len(self.ops[e])
            for d in lasts:
                if d.eng == e and not d.dma:
                    continue
                op.deps.append(d)
                if not d.dma:
                    d.needs_inc = True
            self.ops[e].append(op)
        self.last_w = {}
        self.readers = {}
        if bump:
            self.epoch += 1

    def emit(self, final_waits=True):
        nc = self.nc
        used = set()
        for e in ENGS:
            c = {}
            for op in self.ops[e]:
                if op.dma:
                    continue
                if op.needs_inc:
                    c[op.epoch] = c.get(op.epoch, 0) + 1
                    op.val = c[op.epoch]
                    used.add((e, op.epoch))
        import contextlib

        with contextlib.ExitStack() as st:
            esem = {(e, ep): st.enter_context(nc.semaphore("s_%s_%d" % (e, ep))) for (e, ep) in sorted(used)}
            dsem = [st.enter_context(nc.semaphore("d_%d" % i)) for i in range(NDMASEM)]
            block = st.enter_context(nc.Block())

            def run(e, eng):
                waited = {}
                for op in self.ops[e]:
                    for d in op.deps:
                        if d.dma:
                            key, val, sem = ("d", d.dsem), d.dval, dsem[d.dsem]
                        else:
                            key, val, sem = (d.eng, d.epoch), d.val, esem[(d.eng, d.epoch)]
                        if waited.get(key, 0) >= val:
                            continue
                        eng.wait_ge(sem, val)
                        waited[key] = val
                    if op.fn is None:
                        continue
                    ins = op.fn(eng)
                    if op.dma:
                        ins.then_inc(dsem[op.dsem], 16)
                    elif op.needs_inc:
                        ins.then_inc(esem[(e, op.epoch)], 1)
                if final_waits:
                    for d in self.dma_prev:
                        if d is not None and d.eng == e:
                            if waited.get(("d", d.dsem), 0) < d.dval:
                                eng.wait_ge(dsem[d.dsem], d.dval)

            @block.tensor
            def _(eng):
                run("pe", eng)

            @block.vector
            def _(eng):
                run("dve", eng)

            @block.scalar
            def _(eng):
                run("act", eng)

            @block.gpsimd
            def _(eng):
                run("pool", eng)

            @block.sync
            def _(eng):
                run("sp", eng)


AX = mybir.AxisListType
D = 1024
SEQ = 4096
NCTX = 256
NT = SEQ + NCTX
NL = 2
CH = [(i * 512, 512) for i in range(8)] + [(4096, 256)]
NKB = NT // 128
EPS = 1e-6
ARENA_WORDS = 48500
DEBUG = False


class Arena:
    def __init__(self, nc, words):
        self.t = nc.alloc_sbuf_tensor("arena", [128, words], F32).ap()
        self.words = words
        self.off = 0

    def reset(self, off=0):
        self.off = off

    def tile(self, dtype, shape):
        n = int(np.prod(shape))
        nbytes = n * (2 if dtype == BF16 else 4)
        w = (nbytes + 3) // 4
        w = (w + 7) // 8 * 8
        o = self.off
        self.off += w
        assert self.off <= self.words, "arena overflow %d" % self.off
        v = self.t[:, o:o + w]
        if dtype != F32:
            v = v.bitcast(dtype)
        v = v[:, 0:n]
        if len(shape) == 2:
            v = v.rearrange("p (a b) -> p a b", a=shape[0])
        elif len(shape) == 3:
            v = v.rearrange("p (a b c) -> p a b c", a=shape[0], b=shape[1])
        return v


class K:
    def __init__(self, P):
        self.P = P

    def mm(self, out, lhsT, rhs, start=True, stop=True, reads=(), writes=()):
        return self.P.pe(lambda e: e.matmul(out, lhsT=lhsT, rhs=rhs, start=start, stop=stop), reads, writes)

    def transpose(self, out, in_, ident, reads=(), writes=()):
        return self.P.pe(lambda e: e.transpose(out, in_, ident), reads, writes)

    def act(self, out, in_, func, bias=None, scale=1.0, reads=(), writes=()):
        if bias is None:
            return self.P.act(lambda e: e.activation(out=out, in_=in_, func=func, scale=scale), reads, writes)
        return self.P.act(lambda e: e.activation(out=out, in_=in_, func=func, bias=bias, scale=scale), reads, writes)

    def _eng(self, eng):
        return {"dve": self.P.dve, "pool": self.P.pool}[eng]

    def tt(self, eng, out, in0, in1, op, reads=(), writes=()):
        return self._eng(eng)(lambda e: e.tensor_tensor(out=out, in0=in0, in1=in1, op=op), reads, writes)

    def ts(self, eng, out, in0, s1, op0, s2=None, op1=None, reads=(), writes=()):
        if op1 is None:
            return self._eng(eng)(lambda e: e.tensor_scalar(out=out, in0=in0, scalar1=s1, scalar2=None, op0=op0), reads, writes)
        return self._eng(eng)(lambda e: e.tensor_scalar(out=out, in0=in0, scalar1=s1, scalar2=s2, op0=op0, op1=op1), reads, writes)

    def stt(self, out, in0, scalar, in1, op0, op1, reads=(), writes=()):
        return self.P.dve(lambda e: e.scalar_tensor_tensor(out=out, in0=in0, scalar=scalar, in1=in1, op0=op0, op1=op1), reads, writes)

    def copy(self, eng, out, in_, reads=(), writes=()):
        return self._eng(eng)(lambda e: e.tensor_copy(out=out, in_=in_), reads, writes)

    def recip(self, out, in_, reads=(), writes=()):
        return self.P.dve(lambda e: e.reciprocal(out=out, in_=in_), reads, writes)

    def reduce(self, out, in_, op, axis, reads=(), writes=()):
        return self.P.dve(lambda e: e.tensor_reduce(out=out, in_=in_, axis=axis, op=op), reads, writes)

    def memset(self, eng, ap, val, writes=()):
        return self._eng(eng)(lambda e: e.memset(ap, val), (), writes)

    def dma(self, out, in_, reads=(), writes=(), q="sp"):
        return self.P.dma(lambda e: e.dma_start(out=out, in_=in_), reads, writes, q=q)


def build_program(dbg=False):
    nc = bass.Bass("TRN2", target_bir_lowering=False)
    P = Prog(nc)
    k = K(P)

    def din(name, shape, dt=F32):
        return nc.dram_tensor(name, list(shape), dt, kind="ExternalInput").ap()

    def dscr(name, shape, dt):
        if dbg:
            return nc.dram_tensor(name, list(shape), dt, kind="ExternalOutput").ap()
        return nc.dram_tensor(name, list(shape), dt).ap()

    xT = din("xT", [D, SEQ])
    ctxT = din("ctxT", [D, NCTX])
    cc = din("cc", [128, 16])
    w_mod = din("w_mod", [NL, D, 6 * D])
    bm = din("bm", [NL, 128, 48])
    g1 = din("g1", [NL, 128, 8])
    g2 = din("g2", [NL, 128, 8])
    gf = din("gf", [128, 8])
    w_in = din("w_in", [NL, D, 1696])
    w_in_sw = din("w_in_sw", [NL, D, 1696])
    w_out = din("w_out", [NL, D, D])
    sink = din("sink", [NL, 1, 6])
    gqk = din("gqk", [NL, 64, 4])
    gmq = din("gmq", [NL, 128, 2])
    gmkv = din("gmkv", [NL, 128, 1])
    w_uq = din("w_uq", [NL, 256, 384])
    w_uq_sw = din("w_uq_sw", [NL, 256, 384])
    w_ukv = din("w_ukv", [NL, 128, 512])
    rw = din("rw", [128, 8 * 16])
    rb = din("rb", [16])
    ewg = din("ewg", [NL, 16, D, 512])
    ewu = din("ewu", [NL, 16, D, 512])
    ewd = din("ewd", [NL, 16, 512, D])
    swg = din("swg", [NL, D, 512])
    swu = din("swu", [NL, D, 512])
    swd = din("swd", [NL, 512, D])
    tabs = din("tabs", [4, 128, NT])
    masks = din("masks", [128, 6 * 512])
    ident_d = din("ident", [128, 128])
    outT = nc.dram_tensor("outT", [D, SEQ], F32, kind="ExternalOutput").ap()

    HT = [dscr("HT%d" % l, [8, 128, NT], BF16) for l in range(NL)]
    HT2 = [dscr("HT2_%d" % l, [8, 128, NT], BF16) for l in range(NL)]
    QS = [dscr("QS%d" % l, [16, 96, NT], BF16) for l in range(NL)]
    YS = [dscr("YS%d" % l, [16, 65, NT], BF16) for l in range(NL)]
    X1 = [dscr("X1_%d" % l, [8, 128, NT], F32) for l in range(NL)]
    X2 = [dscr("X2_%d" % l, [8, 128, NT], F32) for l in range(NL)]

    def xsrc(l, c):
        t0, W = CH[c]
        if l == 0:
            if c < 8:
                return xT[:, t0:t0 + W].rearrange("(k p) t -> p k t", p=128)
            return ctxT.rearrange("(k p) t -> p k t", p=128)
        return X2[l - 1][:, :, t0:t0 + W].rearrange("k p t -> p k t")

    def sview(T, c):
        t0, W = CH[c]
        return T[:, :, t0:t0 + W].rearrange("k p t -> p k t")

    def sb(name, shape, dt=F32):
        return nc.alloc_sbuf_tensor(name, list(shape), dt).ap()

    ones_bf = sb("ones_bf", [128, 128], BF16)
    ones_f = sb("ones_f", [128, 128], F32)
    ident = sb("ident_sb", [128, 128], F32)
    epsb = sb("epsb", [128, 1], F32)
    cc_t = sb("cc_t", [128, 16], F32)
    sc_t = sb("sc_t", [128, 16], F32)
    modv = sb("modv", [128, 96], F32)
    bm_t = sb("bm_t", [128, 48], F32)
    g1_t = sb("g1_t", [128, 8], F32)
    g2_t = sb("g2_t", [128, 8], F32)
    gf_t = sb("gf_t", [128, 8], F32)
    A1 = sb("A1", [128, 16], F32)
    A2 = sb("A2", [128, 16], F32)
    gqk_t = sb("gqk_t", [64, 4], F32)
    gmq_t = sb("gmq_t", [128, 2], F32)
    gmkv_t = sb("gmkv_t", [128, 1], F32)
    sink_t = sb("sink_t", [1, 6], F32)
    rw_t = sb("rw_t", [128, 128], F32)
    rb_t = sb("rb_t", [128, 16], F32)
    Gs = sb("Gs", [128, NKB * 16], F32)
    rt = [sb("rt%d" % i, [128, 160], F32) for i in range(2)]

    psbig = nc.alloc_psum_tensor("psbig", [128, 4096], F32).ap()
    ps = [psbig[:, i * 512:(i + 1) * 512] for i in range(8)]
    PSK = [("ps", i) for i in range(8)]

    A = Arena(nc, ARENA_WORDS)

    modv3 = modv.rearrange("p (j c) -> p j c", c=2)
    A13 = A1.rearrange("p (j c) -> p j c", c=2)
    A23 = A2.rearrange("p (j c) -> p j c", c=2)
    sc3 = sc_t.rearrange("p (j c) -> p j c", c=2)
    rw3 = rw_t.rearrange("p (k e) -> p k e", e=16)
    Gs3 = Gs.rearrange("p (t e) -> p t e", e=16)

    k.memset("pool", ones_bf, 1.0, writes=["ones_bf"])
    k.memset("pool", ones_f, 1.0, writes=["ones_f"])
    k.memset("pool", epsb, EPS, writes=["epsb"])
    k.dma(ident, ident_d, writes=["ident"])
    k.dma(cc_t, cc, writes=["cc_t"])
    k.dma(gf_t, gf, writes=["gf_t"])
    k.dma(rw_t, rw, writes=["rw_t"])
    k.dma(rb_t, rb.partition_broadcast(128), writes=["rb_t"])
    k.act(sc_t, cc_t, AF.Silu, reads=["cc_t"], writes=["sc_t"])


    def phase_barrier(bump=True):
        P.barrier(bump)

    def norm_mod(xc, W, Acol, Scol, sq, lnv, rstd, tmp, out, out_is_f32, kx, kout, pz, pzk, scale_in=1.0 / D):
        k.act(sq[:, :, :W], xc[:, :, :W], AF.Square, reads=[kx], writes=["nm_sq"])
        for kc in range(8):
            k.mm(pz[:, :W], ones_bf, sq[:, kc, :W], start=(kc == 0), stop=(kc == 7),
                 reads=["nm_sq", "ones_bf"], writes=[pzk])
        k.act(lnv[:, :W], pz[:, :W], AF.Ln, bias=epsb[:, 0:1], scale=scale_in, reads=[pzk, "epsb"], writes=["nm_ln"])
        k.act(rstd[:, :W], lnv[:, :W], AF.Exp, scale=-0.5, reads=["nm_ln"], writes=["nm_rstd"])
        for kc in range(8):
            k.stt(tmp[:, kc, :W], xc[:, kc, :W], Acol[:, kc:kc + 1], rstd[:, :W], ALU.mult, ALU.mult,
                  reads=[kx, "nm_rstd", "modc"], writes=[("nm_tmp", kc)])
            k.act(out[:, kc, :W], tmp[:, kc, :W], AF.Identity, bias=Scol[:, kc:kc + 1], scale=1.0,
                  reads=[("nm_tmp", kc), "modc"], writes=[kout])

    stg_ctr = [0]

    def load_cast(stg, dst, src, key, np_=128, eng="act"):
        Kk, cols = dst.shape[1], dst.shape[2]
        cb = max(1, int(stg[0].shape[1]) // Kk)
        for c0 in range(0, cols, cb):
            c1 = min(cols, c0 + cb)
            i = stg_ctr[0] % len(stg)
            stg_ctr[0] += 1
            sv = stg[i][0:np_, 0:Kk * (c1 - c0)].rearrange("p (k c) -> p k c", k=Kk)
            srcv = src[:, c0:c1].rearrange("(k p) c -> p k c", p=np_)
            k.dma(sv, srcv, writes=[("stg", i)])
            if eng == "act":
                k.act(dst[0:np_, :, c0:c1], sv, AF.Copy, reads=[("stg", i)], writes=[key])
            else:
                k.copy(eng, dst[0:np_, :, c0:c1], sv, reads=[("stg", i)], writes=[key])

    for l in range(NL):
        last = (l == NL - 1)
        phase_barrier()
        A.reset()
        k.dma(bm_t, bm[l], writes=["bm_t"])
        k.dma(g1_t, g1[l], writes=["g1_t"])
        k.dma(g2_t, g2[l], writes=["g2_t"])
        k.dma(gqk_t, gqk[l], writes=["gqk_t"])
        k.dma(gmq_t, gmq[l], writes=["gmq_t"])
        k.dma(gmkv_t, gmkv[l], writes=["gmkv_t"])
        k.dma(sink_t, sink[l], writes=["sink_t"])
        k.act(sink_t, sink_t, AF.Exp, reads=["sink_t"], writes=["sink_t"])
        wst = [A.tile(F32, [8, 512]) for _ in range(2)]
        for j in range(12):
            wv = wst[j % 2]
            k.dma(wv, w_mod[l][:, j * 512:(j + 1) * 512].rearrange("(k p) c -> p k c", p=128), writes=[("wst", j % 2)])
            for jj in range(4):
                f = j * 4 + jj
                for kc in range(8):
                    k.mm(ps[7][:, 2 * f:2 * f + 2], wv[:, kc, jj * 128:(jj + 1) * 128], sc3[:, kc, :],
                         start=(kc == 0), stop=(kc == 7), reads=[("wst", j % 2), "sc_t"], writes=[PSK[7]])
        k.tt("dve", modv3, ps[7][:, 0:96].rearrange("p (j c) -> p j c", c=2),
             bm_t.unsqueeze(2).broadcast_to([128, 48, 2]), ALU.add, reads=[PSK[7], "bm_t"], writes=["modc"])
        k.stt(A13, modv3[:, 8:16, :], 1.0, g1_t.unsqueeze(2).broadcast_to([128, 8, 2]), ALU.add, ALU.mult,
              reads=["modc", "g1_t"], writes=["modc"])
        k.stt(A23, modv3[:, 32:40, :], 1.0, g2_t.unsqueeze(2).broadcast_to([128, 8, 2]), ALU.add, ALU.mult,
              reads=["modc", "g2_t"], writes=["modc"])

        phase_barrier(False)
        A.reset()
        xb = [A.tile(F32, [8, 512]) for _ in range(2)]
        hb = [A.tile(BF16, [8, 512]) for _ in range(2)]
        sq = A.tile(BF16, [8, 512])
        tmp = A.tile(F32, [8, 512])
        lnv = A.tile(F32, [512])
        rstd = A.tile(F32, [512])
        chunks = list(range(9))
        for c in chunks:
            t0, W = CH[c]
            col = 1 if c == 8 else 0
            xc = xb[c % 2]
            k.dma(xc[:, :, :W], xsrc(l, c), writes=[("xb", c % 2)])
            norm_mod(xc, W, A13[:, :, col], modv3[:, 0:8, col], sq, lnv, rstd, tmp, hb[c % 2], False,
                     ("xb", c % 2), ("hb", c % 2), ps[6], PSK[6])
            k.dma(sview(HT[l], c), hb[c % 2][:, :, :W], reads=[("hb", c % 2)], writes=[("HT", c)])

        def rope_out(M, r0, W, a_ps, b_ps, ak, bk, Ct, St, tk, out_ap, wkey, t1, t2, gain=None, rr=None, t3=None, sx=0):
            k1, k2, k3, kr = ("t1", sx), ("t2", sx), ("t3", sx), ("rr", sx)
            if gain is None:
                k.tt("dve", t1[r0:M, :W], a_ps[r0:M, :W], Ct[r0:M, :W], ALU.mult, reads=[ak, tk], writes=[k1])
                k.tt("dve", t2[r0:M, :W], b_ps[r0:M, :W], St[r0:M, :W], ALU.mult, reads=[bk, tk], writes=[k2])
                k.tt("pool", out_ap, t1[r0:M, :W], t2[r0:M, :W], ALU.add, reads=[k1, k2, "KTz"], writes=[wkey])
            else:
                k.stt(t1[r0:M, :W], a_ps[r0:M, :W], gain[0], Ct[r0:M, :W], ALU.mult, ALU.mult,
                      reads=[ak, tk, "gqk_t"], writes=[k1])
                k.stt(t2[r0:M, :W], b_ps[r0:M, :W], gain[1], St[r0:M, :W], ALU.mult, ALU.mult,
                      reads=[bk, tk, "gqk_t"], writes=[k2])
                k.tt("pool", t3[r0:M, :W], t1[r0:M, :W], t2[r0:M, :W], ALU.add, reads=[k1, k2], writes=[k3])
                k.tt("pool", out_ap, t3[r0:M, :W], rr[r0:M, :W], ALU.mult, reads=[k3, kr, "KTz"], writes=[wkey])

        pend = [None]
        actr = [0]

        def flush_pend():
            if pend[0] is not None:
                fn_ = pend[0]
                pend[0] = None
                fn_()

        def attention(hg, dk, KTh, Vh, scale, typ, sink_idx, qb, ptb, den, rdenb, bcs, yst, msk, otsb):
            qchunks = list(range(8)) + ([8] if not last else [])
            ot, otk = ps[6], PSK[6]
            for c in qchunks:
                it = actr[0]
                actr[0] += 1
                t0, W = CH[c]
                qt = qb[it % 2]
                k.dma(qt[0:dk, :W], QS[l][hg, 0:dk, t0:t0 + W], reads=[("QS", hg, c)], writes=[("qb", it % 2)])
                if c == 8:
                    kbs = [(32, None), (33, None)]
                elif typ == "swa":
                    kbs = [(32, None), (33, None)] + [(j, j - 4 * c) for j in range(4 * c - 1, 4 * c + 5) if 0 <= j < 32]
                else:
                    kbs = [(j, None) for j in range(NKB)]
                groups = [kbs[i:i + 2] for i in range(0, len(kbs), 2)]

                def qk_group(gi):
                    b = (gi % 3) * 2
                    for j, (kb, rel) in enumerate(groups[gi]):
                        k.mm(ps[b + j][:, :W], KTh[:, kb * 128:(kb + 1) * 128], qt[:, :W],
                             reads=[("qb", it % 2)], writes=[PSK[b + j]])

                qk_group(0)
                if len(groups) > 1:
                    qk_group(1)
                n = 0
                for gi, g in enumerate(groups):
                    if gi + 2 < len(groups):
                        qk_group(gi + 2)
                    if gi == 1:
                        flush_pend()
                    b = (gi % 3) * 2
                    ng = len(g)
                    pt = ptb[gi % 3]
                    src = psbig[:, b * 512:(b + ng) * 512].rearrange("p (a t) -> p a t", a=ng)[:, :, :W]
                    k.act(pt[:, 0:ng, :W], src, AF.Exp, scale=scale, reads=[PSK[b + j] for j in range(ng)],
                          writes=[("pt", gi % 3)])
                    for j, (kb, rel) in enumerate(g):
                        if rel is not None:
                            k.tt("pool" if n % 2 == 0 else "dve", pt[:, j, :W], pt[:, j, :W], msk[:, rel + 1, :W], ALU.mult,
                                 reads=[("pt", gi % 3), "msk"], writes=[("pt", gi % 3)])
                        k.mm(ot[0:65, :W], Vh[:, kb, :], pt[:, j, :W], start=(n == 0), stop=(n == len(kbs) - 1),
                             reads=[("pt", gi % 3)], writes=[otk])
                        n += 1
                flush_pend()
                osb = otsb[it % 2]
                rd = rdenb[it % 2]
                k.copy("dve", osb[0:65, :W], ot[0:65, :W], reads=[otk], writes=[("otsb", it % 2)])
                if sink_idx is not None:
                    k.ts("dve", den[0:1, :W], osb[0:1, :W], sink_t[0:1, sink_idx:sink_idx + 1], ALU.add,
                         reads=[("otsb", it % 2), "sink_t"], writes=["den"])
                    k.recip(rd[0:1, :W], den[0:1, :W], reads=["den"], writes=[("rden", it % 2)])
                else:
                    k.recip(rd[0:1, :W], osb[0:1, :W], reads=[("otsb", it % 2)], writes=[("rden", it % 2)])

                def e2(osb=osb, rd=rd, it=it, hg=hg, t0=t0, W=W, c=c):
                    k.mm(ps[7][0:65, :W], ones_f[0:1, 0:65], rd[0:1, :W], reads=[("rden", it % 2), "ones_f"], writes=[PSK[7]])
                    k.act(bcs[0:65, :W], ps[7][0:65, :W], AF.Copy, reads=[PSK[7]], writes=["bcs"])
                    ys = yst[it % 2]
                    k.tt("dve", ys[0:65, :W], osb[0:65, :W], bcs[0:65, :W], ALU.mult, reads=[("otsb", it % 2), "bcs"],
                         writes=[("yst", it % 2)])
                    k.dma(YS[l][hg, :, t0:t0 + W], ys[0:65, :W], reads=[("yst", it % 2)], writes=[("YS", c)])

                pend[0] = e2

        for typ in ("swa", "glb", "mla"):
            phase_barrier()
            A.reset()
            nkv = 4 if typ == "mla" else 2
            dk = 96 if typ == "mla" else 64
            KT = A.tile(BF16, [nkv, NT])
            V = A.tile(BF16, [nkv, NKB, 65])
            mark = A.off
            stg = [A.tile(F32, [4096]) for _ in range(2)]
            hb = [A.tile(BF16, [8, 512]) for _ in range(2)]
            tb = [A.tile(F32, [2, 512]) for _ in range(2)]
            t1s = [A.tile(F32, [512]) for _ in range(2)]
            t2s = [A.tile(F32, [512]) for _ in range(2)]
            t3s = [A.tile(F32, [512]) for _ in range(2)]
            rrs = [A.tile(F32, [512]) for _ in range(2)]
            lnvs = [A.tile(F32, [512]) for _ in range(2)]
            sqhs = [A.tile(BF16, [2, 512]) for _ in range(2)]
            t1, t2, t3, rr, lnv, sqh = t1s[0], t2s[0], t3s[0], rrs[0], lnvs[0], sqhs[0]
            qst = [A.tile(BF16, [512]) for _ in range(2)]
            k.memset("pool", V[:, :, :, 0:1], 1.0, writes=["Vones"])
            k.memset("pool", KT, 0.0, writes=["KTz"])
            if typ != "mla":
                base = 0 if typ == "swa" else 640
                hbase = 0 if typ == "swa" else 6
                wq = A.tile(BF16, [8, 384])
                wqs = A.tile(BF16, [8, 384])
                wk = A.tile(BF16, [8, 128])
                wks = A.tile(BF16, [8, 128])
                wv = A.tile(BF16, [8, 128])
                load_cast(stg, wq, w_in[l][:, base:base + 384], "W")
                load_cast(stg, wqs, w_in_sw[l][:, base:base + 384], "W")
                load_cast(stg, wk, w_in[l][:, base + 384:base + 512], "W")
                load_cast(stg, wks, w_in_sw[l][:, base + 384:base + 512], "W")
                load_cast(stg, wv, w_in[l][:, base + 512:base + 640], "W")
                tix = 0
                for c in range(9):
                    t0, W = CH[c]
                    hc = hb[c % 2]
                    k.dma(hc[:, :, :W], sview(HT[l], c), reads=[("HT", c)], writes=[("hb", c % 2)])
                    tc_ = tb[c % 2]
                    k.dma(tc_[0:64, :, :W], tabs[0:2, 0:64, t0:t0 + W].rearrange("a p t -> p a t"), writes=[("tb", c % 2)])
                    Ct, St = tc_[:, 0, :], tc_[:, 1, :]
                    jobs = [("k", kv) for kv in range(2)]
                    if not (last and c == 8):
                        jobs += [("q", h) for h in range(6)]
                    for kind, idx in jobs:
                        sx = tix % 2
                        t1, t2, t3, rr, lnv, sqh = t1s[sx], t2s[sx], t3s[sx], rrs[sx], lnvs[sx], sqhs[sx]
                        wa, wb = (wk, wks) if kind == "k" else (wq, wqs)
                        pa, pb = ps[(tix % 2) * 2], ps[(tix % 2) * 2 + 1]
                        pak, pbk = PSK[(tix % 2) * 2], PSK[(tix % 2) * 2 + 1]
                        for kc in range(8):
                            k.mm(pa[0:64, :W], wa[:, kc, idx * 64:(idx + 1) * 64], hc[:, kc, :W], start=(kc == 0),
                                 stop=(kc == 7), reads=["W", ("hb", c % 2)], writes=[pak])
                        for kc in range(8):
                            k.mm(pb[0:64, :W], wb[:, kc, idx * 64:(idx + 1) * 64], hc[:, kc, :W], start=(kc == 0),
                                 stop=(kc == 7), reads=["W", ("hb", c % 2)], writes=[pbk])
                        if kind == "k":
                            out_ap, wkey = KT[0:64, idx, t0:t0 + W], ("KT", idx, c)
                        else:
                            out_ap, wkey = qst[tix % 2][0:64, :W], ("qst", tix % 2)
                        if typ == "glb":
                            go = 0 if kind == "q" else 2
                            k.act(sqh[0:64, 0, :W], pa[0:64, :W], AF.Square, reads=[pak], writes=[("sqh", sx)])
                            k.mm(ps[6][0:64, :W], ones_bf[0:64, 0:64], sqh[0:64, 0, :W], reads=[("sqh", sx), "ones_bf"], writes=[PSK[6]])
                            k.act(lnv[0:64, :W], ps[6][0:64, :W], AF.Ln, bias=epsb[0:64, 0:1], scale=1.0 / 64,
                                  reads=[PSK[6], "epsb"], writes=[("lnv", sx)])
                            k.act(rr[0:64, :W], lnv[0:64, :W], AF.Exp, scale=-0.5, reads=[("lnv", sx)], writes=[("rr", sx)])
                            rope_out(64, 0, W, pa, pb, pak, pbk, Ct, St, ("tb", c % 2), out_ap, wkey, t1, t2,
                                     gain=(gqk_t[:, go:go + 1], gqk_t[:, go + 1:go + 2]), rr=rr, t3=t3, sx=sx)
                        else:
                            rope_out(64, 0, W, pa, pb, pak, pbk, Ct, St, ("tb", c % 2), out_ap, wkey, t1, t2, sx=sx)
                        if kind == "q":
                            k.dma(QS[l][hbase + idx, 0:64, t0:t0 + W], out_ap, reads=[wkey], writes=[("QS", hbase + idx, c)])
                        tix += 1
                    for tt_ in range(W // 128):
                        kb = t0 // 128 + tt_
                        for kc in range(8):
                            k.mm(ps[5][:, 0:128], hc[:, kc, tt_ * 128:(tt_ + 1) * 128], wv[:, kc, :], start=(kc == 0),
                                 stop=(kc == 7), reads=["W", ("hb", c % 2)], writes=[PSK[5]])
                        k.copy("dve", V[:, :, kb, 1:65], ps[5][:, 0:128].rearrange("p (a d) -> p a d", a=2),
                               reads=[PSK[5], "Vones"], writes=[("V", kb)])
            else:
                wql = A.tile(BF16, [8, 256])
                wkvl = A.tile(BF16, [8, 128])
                wkr = A.tile(BF16, [8, 96])
                wkrs = A.tile(BF16, [8, 96])
                wuq = A.tile(BF16, [2, 384])
                wuqs = A.tile(BF16, [2, 384])
                wukv = A.tile(BF16, [1, 512])
                latq = A.tile(BF16, [2, 512])
                latkv = A.tile(BF16, [512])
                k.memset("pool", wkr, 0.0, writes=["W"])
                k.memset("pool", wkrs, 0.0, writes=["W"])
                load_cast(stg, wql, w_in[l][:, 1280:1536], "W")
                load_cast(stg, wkvl, w_in[l][:, 1536:1664], "W")
                load_cast(stg, wkr[:, :, 64:96], w_in[l][:, 1664:1696], "W")
                load_cast(stg, wkrs[:, :, 64:96], w_in_sw[l][:, 1664:1696], "W")
                load_cast(stg, wuq, w_uq[l], "W")
                load_cast(stg, wuqs, w_uq_sw[l], "W")
                load_cast(stg, wukv, w_ukv[l], "W")
                tix = 0
                for c in range(9):
                    t0, W = CH[c]
                    hc = hb[c % 2]
                    k.dma(hc[:, :, :W], sview(HT[l], c), reads=[("HT", c)], writes=[("hb", c % 2)])
                    tc_ = tb[c % 2]
                    k.dma(tc_[0:96, :, :W], tabs[2:4, 0:96, t0:t0 + W].rearrange("a p t -> p a t"), writes=[("tb", c % 2)])
                    Ct, St = tc_[:, 0, :], tc_[:, 1, :]
                    needq = not (last and c == 8)
                    if needq:
                        for kc2 in range(2):
                            for kc in range(8):
                                k.mm(ps[kc2][:, :W], wql[:, kc, kc2 * 128:(kc2 + 1) * 128], hc[:, kc, :W], start=(kc == 0),
                                     stop=(kc == 7), reads=["W", ("hb", c % 2)], writes=[PSK[kc2]])
                            k.act(sqh[:, kc2, :W], ps[kc2][:, :W], AF.Square, reads=[PSK[kc2]], writes=[("sqh", kc2)])
                        for kc2 in range(2):
                            k.mm(ps[6][:, :W], ones_bf, sqh[:, kc2, :W], start=(kc2 == 0), stop=(kc2 == 1),
                                 reads=[("sqh", kc2), "ones_bf"], writes=[PSK[6]])
                        k.act(lnv[:, :W], ps[6][:, :W], AF.Ln, bias=epsb[:, 0:1], scale=1.0 / 256, reads=[PSK[6], "epsb"], writes=["lnv"])
                        k.act(rr[:, :W], lnv[:, :W], AF.Exp, scale=-0.5, reads=["lnv"], writes=["rr"])
                        for kc2 in range(2):
                            k.stt(latq[:, kc2, :W], ps[kc2][:, :W], gmq_t[:, kc2:kc2 + 1], rr[:, :W], ALU.mult, ALU.mult,
                                  reads=[PSK[kc2], "rr", "gmq_t"], writes=["latq"])
                    for kc in range(8):
                        k.mm(ps[2][:, :W], wkvl[:, kc, :], hc[:, kc, :W], start=(kc == 0), stop=(kc == 7),
                             reads=["W", ("hb", c % 2)], writes=[PSK[2]])
                    k.act(sqh[:, 0, :W], ps[2][:, :W], AF.Square, reads=[PSK[2]], writes=[("sqh", 0)])
                    k.mm(ps[6][:, :W], ones_bf, sqh[:, 0, :W], reads=[("sqh", 0), "ones_bf"], writes=[PSK[6]])
                    k.act(lnv[:, :W], ps[6][:, :W], AF.Ln, bias=epsb[:, 0:1], scale=1.0 / 128, reads=[PSK[6], "epsb"], writes=["lnv"])
                    k.act(rr[:, :W], lnv[:, :W], AF.Exp, scale=-0.5, reads=["lnv"], writes=["rr"])
                    k.stt(latkv[:, :W], ps[2][:, :W], gmkv_t[:, 0:1], rr[:, :W], ALU.mult, ALU.mult,
                          reads=[PSK[2], "rr", "gmkv_t"], writes=["latkv"])
                    for kc in range(8):
                        k.mm(ps[0][0:96, :W], wkr[:, kc, :], hc[:, kc, :W], start=(kc == 0), stop=(kc == 7),
                             reads=["W", ("hb", c % 2)], writes=[PSK[0]])
                    for kc in range(8):
                        k.mm(ps[1][0:96, :W], wkrs[:, kc, :], hc[:, kc, :W], start=(kc == 0), stop=(kc == 7),
                             reads=["W", ("hb", c % 2)], writes=[PSK[1]])
                    rope_out(96, 64, W, ps[0], ps[1], PSK[0], PSK[1], Ct, St, ("tb", c % 2), KT[64:96, 0, t0:t0 + W], ("KTr", 0, c), t1, t2)
                    for h in range(1, 4):
                        k.copy("pool", KT[64:96, h, t0:t0 + W], KT[64:96, 0, t0:t0 + W], reads=[("KTr", 0, c)], writes=[("KTr", h, c)])
                    for h in range(4):
                        k.mm(ps[3][0:64, :W], wukv[:, 0, h * 128:h * 128 + 64], latkv[:, :W], reads=["W", "latkv"], writes=[PSK[3]])
                        k.act(KT[0:64, h, t0:t0 + W], ps[3][0:64, :W], AF.Copy, reads=[PSK[3], "KTz"], writes=[("KTn", h, c)])
                        if needq:
                            pa, pb = ps[(tix % 2) * 2], ps[(tix % 2) * 2 + 1]
                            pak, pbk = PSK[(tix % 2) * 2], PSK[(tix % 2) * 2 + 1]
                            for kc2 in range(2):
                                k.mm(pa[0:96, :W], wuq[:, kc2, h * 96:(h + 1) * 96], latq[:, kc2, :W], start=(kc2 == 0),
                                     stop=(kc2 == 1), reads=["W", "latq"], writes=[pak])
                            for kc2 in range(2):
                                k.mm(pb[0:96, :W], wuqs[:, kc2, h * 96:(h + 1) * 96], latq[:, kc2, :W], start=(kc2 == 0),
                                     stop=(kc2 == 1), reads=["W", "latq"], writes=[pbk])
                            out_ap, wkey = qst[tix % 2][0:96, :W], ("qst", tix % 2)
                            rope_out(96, 0, W, pa, pb, pak, pbk, Ct, St, ("tb", c % 2), out_ap, wkey, t1, t2)
                            k.dma(QS[l][12 + h, 0:96, t0:t0 + W], out_ap, reads=[wkey], writes=[("QS", 12 + h, c)])
                            tix += 1
                    wv4 = wukv[:, 0, :].rearrange("p (h d) -> p h d", h=4)[:, :, 64:128]
                    for tt_ in range(W // 128):
                        kb = t0 // 128 + tt_
                        k.mm(ps[5][:, 0:256].rearrange("p (h d) -> p h d", h=4), latkv[:, tt_ * 128:(tt_ + 1) * 128], wv4,
                             reads=["W", "latkv"], writes=[PSK[5]])
                        k.copy("dve", V[:, :, kb, 1:65], ps[5][:, 0:256].rearrange("p (a d) -> p a d", a=4),
                               reads=[PSK[5], "Vones"], writes=[("V", kb)])
            phase_barrier(typ == "glb")
            A.reset(mark)
            qb = [A.tile(BF16, [512]) for _ in range(2)]
            for i_ in range(2):
                k.memset("pool", qb[i_], 0.0, writes=[("qb", i_)])
            ptb = [A.tile(BF16, [2, 512]) for _ in range(3)]
            otsb = [A.tile(F32, [512]) for _ in range(2)]
            rdenb = [A.tile(F32, [512]) for _ in range(2)]
            yst = [A.tile(BF16, [512]) for _ in range(2)]
            den = A.tile(F32, [512])
            rden = A.tile(F32, [512])
            bcs = A.tile(F32, [512])
            msk = A.tile(BF16, [6, 512])
            if typ == "swa":
                mst = A.tile(F32, [6, 512])
                k.dma(mst, masks.rearrange("p (a t) -> p a t", a=6), writes=["mst"])
                k.copy("pool", msk, mst, reads=["mst"], writes=["msk"])
            nh = 4 if typ == "mla" else 6
            hbase = {"swa": 0, "glb": 6, "mla": 12}[typ]
            for h in range(nh):
                kvh = h if typ == "mla" else h // 3
                scale = (96 ** -0.5) if typ == "mla" else 0.125
                attention(hbase + h, dk, KT[:, kvh, :], V[:, kvh, :, :], scale, typ, h if typ == "swa" else None,
                          qb, ptb, den, rdenb, bcs, yst, msk, otsb)
            flush_pend()

        phase_barrier()
        A.reset()
        nch = 8 if last else 9
        stg = [A.tile(F32, [4096]) for _ in range(1)]
        wo = A.tile(BF16, [16, 1024])
        k.memset("pool", wo, 0.0, writes=["wo"])
        for hh in range(0, 16, 4):
            sv = stg[0][:, 0:4096].rearrange("p (h c) -> p h c", h=4)
            k.memset("pool", sv[0:1, :, :], 0.0, writes=[("stg", 0)])
            k.dma(sv[1:65, :, :], w_out[l][hh * 64:(hh + 4) * 64, :].rearrange("(h r) c -> r h c", r=64), writes=[("stg", 0)])
            k.act(wo[0:65, hh:hh + 4, :], sv[0:65, :, :], AF.Copy, reads=[("stg", 0)], writes=["wo"])
        ytb = [A.tile(BF16, [16, 512]) for _ in range(1)]
        k.memset("pool", ytb[0], 0.0, writes=[("ytb", 0)])
        xb = [A.tile(F32, [8, 512]) for _ in range(2)]
        x1bs = [A.tile(F32, [8, 512]) for _ in range(2)]
        h32 = A.tile(F32, [8, 512])
        h2b = [A.tile(BF16, [8, 512]) for _ in range(2)]
        sq = A.tile(BF16, [8, 512])
        tmp = A.tile(F32, [8, 512])
        lnv = A.tile(F32, [512])
        rstd = A.tile(F32, [512])
        tile_i = 0
        for c in range(nch):
            t0, W = CH[c]
            col = 1 if c == 8 else 0
            yt = ytb[0]
            x1b = x1bs[c % 2]
            x1k = ("x1b", c % 2)
            k.dma(yt[0:65, :, :W], YS[l][:, :, t0:t0 + W].rearrange("h r t -> r h t"), reads=[("YS", c)], writes=[("ytb", 0)])
            xc = xb[c % 2]
            k.dma(xc[:, :, :W], xsrc(l, c), writes=[("xb", c % 2)])
            for dc in range(8):
                po, pok = ps[dc % 2], PSK[dc % 2]
                for hh in range(16):
                    k.mm(po[:, :W], wo[:, hh, dc * 128:(dc + 1) * 128], yt[:, hh, :W], start=(hh == 0), stop=(hh == 15),
                         reads=["wo", ("ytb", 0)], writes=[pok])
                k.stt(x1b[:, dc, :W], po[:, :W], modv3[:, 16 + dc, col:col + 1], xc[:, dc, :W], ALU.mult, ALU.add,
                      reads=[pok, ("xb", c % 2), "modc"], writes=[x1k])
            k.dma(sview(X1[l], c), x1b[:, :, :W], reads=[x1k], writes=[("X1", c)])
            norm_mod(x1b, W, A23[:, :, col], modv3[:, 24:32, col], sq, lnv, rstd, tmp, h32, True,
                     x1k, "h32", ps[6], PSK[6])
            h2 = h2b[c % 2]
            k.act(h2[:, :, :W], h32[:, :, :W], AF.Copy, reads=["h32"], writes=[("h2b", c % 2)])
            k.dma(sview(HT2[l], c), h2[:, :, :W], reads=[("h2b", c % 2)], writes=[("HT2", c)])
            for tt_ in range(W // 128):
                R = rt[tile_i % 2]
                rk = ("rt", tile_i % 2)
                lg = ps[4 + tile_i % 2][:, 0:16]
                lgk = PSK[4 + tile_i % 2]
                for kc in range(8):
                    k.mm(lg, h32[:, kc, tt_ * 128:(tt_ + 1) * 128], rw3[:, kc, :], start=(kc == 0), stop=(kc == 7),
                         reads=["h32", "rw_t"], writes=[lgk])
                e1, scv, bs, eq, b2, ge, selv, gu, G = [R[:, i * 16:(i + 1) * 16] for i in range(9)]
                m1, m2, gs, gsel = [R[:, 144 + i * 4:148 + i * 4] for i in range(4)]
                v3 = lambda ap: ap.rearrange("p (g e) -> p g e", e=4)
                bc4 = lambda ap: ap.unsqueeze(2).broadcast_to([128, 4, 4])
                k.act(e1, lg, AF.Exp, scale=-1.0, reads=[lgk], writes=[rk])
                k.ts("dve", e1, e1, 1.0, ALU.add, reads=[rk], writes=[rk])
                k.recip(scv, e1, reads=[rk], writes=[rk])
                k.tt("dve", bs, scv, rb_t, ALU.add, reads=[rk, "rb_t"], writes=[rk])
                k.reduce(m1, v3(bs), ALU.max, AX.X, reads=[rk], writes=[rk])
                k.tt("dve", v3(eq), v3(bs), bc4(m1), ALU.is_equal, reads=[rk], writes=[rk])
                k.stt(v3(b2), v3(eq), -1e9, v3(bs), ALU.mult, ALU.add, reads=[rk], writes=[rk])
                k.reduce(m2, v3(b2), ALU.max, AX.X, reads=[rk], writes=[rk])
                k.tt("dve", gs, m1, m2, ALU.add, reads=[rk], writes=[rk])
                k.reduce(e1[:, 0:1], gs, ALU.max, AX.X, reads=[rk], writes=[rk])
                k.ts("dve", gsel, gs, e1[:, 0:1], ALU.is_equal, reads=[rk], writes=[rk])
                k.tt("dve", v3(ge), v3(bs), bc4(m2), ALU.is_ge, reads=[rk], writes=[rk])
                k.tt("dve", v3(selv), v3(ge), bc4(gsel), ALU.mult, reads=[rk], writes=[rk])
                k.tt("dve", gu, scv, selv, ALU.mult, reads=[rk], writes=[rk])
                k.reduce(e1[:, 1:2], gu, ALU.add, AX.X, reads=[rk], writes=[rk])
                k.recip(e1[:, 2:3], e1[:, 1:2], reads=[rk], writes=[rk])
                tok_tile = t0 // 128 + tt_
                k.ts("dve", Gs3[:, tok_tile, :], gu, e1[:, 2:3], ALU.mult, reads=[rk], writes=[("Gs", tok_tile)])
                tile_i += 1

        phase_barrier()
        A.reset()
        scs = [[0, 1, 2], [3, 4, 5], [6, 7] if last else [6, 7, 8]]
        stg = [A.tile(F32, [2048]) for _ in range(3)]
        diag = A.tile(F32, [4, 128])
        wgb = [A.tile(BF16, [8, 512]) for _ in range(2)]
        wub = [A.tile(BF16, [8, 512]) for _ in range(2)]
        wdb = [A.tile(BF16, [4, 1024]) for _ in range(2)]
        h2b = [A.tile(BF16, [8, 512]) for _ in range(2)]
        actb = [A.tile(BF16, [4, 512]) for _ in range(2)]
        acc = A.tile(F32, [8, 1536])
        bcs = A.tile(F32, [512])
        sgb = [A.tile(F32, [512]) for _ in range(2)]
        ugb = [A.tile(BF16, [512]) for _ in range(2)]
        x1c = A.tile(F32, [8, 512])
        sq = A.tile(BF16, [8, 512])
        lnv = A.tile(F32, [512])
        rstd = A.tile(F32, [512])
        items = [(si, e) for si in range(len(scs)) for e in range(17)]
        steps = []
        for n_, (si, e) in enumerate(items):
            o = 0
            for sidx, c in enumerate(scs[si]):
                steps.append(dict(n=n_, si=si, e=e, c=c, sidx=sidx, nsteps=len(scs[si]), k=len(steps), off=o))
                o += CH[c][1]

        def piece(n_, p):
            si, e = items[n_]
            b = n_ % 2
            srcs = (ewg[l, e], ewu[l, e], ewd[l, e]) if e < 16 else (swg[l], swu[l], swd[l])
            if p < 2:
                dst = wgb[b][:, :, p * 256:(p + 1) * 256]
                src = srcs[0][:, p * 256:(p + 1) * 256].rearrange("(k p) c -> p k c", p=128)
                Kk = 8
            elif p < 4:
                q = p - 2
                dst = wub[b][:, :, q * 256:(q + 1) * 256]
                src = srcs[1][:, q * 256:(q + 1) * 256].rearrange("(k p) c -> p k c", p=128)
                Kk = 8
            else:
                q = p - 4
                dst = wdb[b][:, :, q * 512:(q + 1) * 512]
                src = srcs[2][:, q * 512:(q + 1) * 512].rearrange("(k p) c -> p k c", p=128)
                Kk = 4
            sv = stg[p % 3].rearrange("p (k c) -> p k c", k=Kk)
            return dst, src, sv, ("wexp", b, p)

        def piece_dma(n_, p):
            dst, src, sv, key = piece(n_, p)
            k.dma(sv, src, writes=[("stg", p % 3)])

        def piece_cast(n_, p):
            dst, src, sv, key = piece(n_, p)
            k.act(dst, sv, AF.Copy, reads=[("stg", p % 3)], writes=[key])

        def h2_dma(st):
            t0, W = CH[st["c"]]
            kk = st["k"]
            k.dma(h2b[kk % 2][:, :, :W], sview(HT2[l], st["c"]), reads=[("HT2", st["c"])], writes=[("h2b", kk % 2)])

        def gu_part(st):
            n_, e, c, sidx, nsteps, kk = st["n"], st["e"], st["c"], st["sidx"], st["nsteps"], st["k"]
            t0, W = CH[c]
            b = n_ % 2
            wg, wu = wgb[b], wub[b]
            pre = n_ + 1 < len(items)
            if pre:
                if nsteps == 3:
                    if sidx == 0:
                        for p in (0, 1, 2):
                            piece_dma(n_ + 1, p)
                    elif sidx == 1:
                        for p in (0, 1, 2):
                            piece_cast(n_ + 1, p)
                        for p in (3, 4, 5):
                            piece_dma(n_ + 1, p)
                    else:
                        for p in (3, 4, 5):
                            piece_cast(n_ + 1, p)
                else:
                    for p in ((0, 1, 2) if sidx == 0 else (3, 4, 5)):
                        piece_dma(n_ + 1, p)
            if kk + 1 < len(steps):
                h2_dma(steps[kk + 1])
            h2 = h2b[kk % 2]
            hk = ("h2b", kk % 2)
            if e < 16:
                for tt_ in range(W // 128):
                    tok_tile = t0 // 128 + tt_
                    k.ts("dve", diag[:, tt_, :], ident, Gs3[:, tok_tile, e:e + 1], ALU.mult,
                         reads=["ident"], writes=[("diag", tt_)])
                    k.mm(ps[6][:, tt_ * 128:(tt_ + 1) * 128], ones_f, diag[:, tt_, :],
                         reads=[("diag", tt_), "ones_f"], writes=[PSK[6]])
                k.act(bcs[:, :W], ps[6][:, :W], AF.Copy, reads=[PSK[6]], writes=["bcs"])
            act_t = actb[kk % 2]
            ak = ("act", kk % 2)
            for fc in range(4):
                pg, pgk = ps[fc % 2], PSK[fc % 2]
                pu, puk = ps[2 + fc % 2], PSK[2 + fc % 2]
                wgk = ("wexp", b, 0 if fc < 2 else 1)
                wuk = ("wexp", b, 2 if fc < 2 else 3)
                for kc in range(8):
                    k.mm(pg[:, :W], wg[:, kc, fc * 128:(fc + 1) * 128], h2[:, kc, :W], start=(kc == 0), stop=(kc == 7),
                         reads=[wgk, hk], writes=[pgk])
                for kc in range(8):
                    k.mm(pu[:, :W], wu[:, kc, fc * 128:(fc + 1) * 128], h2[:, kc, :W], start=(kc == 0), stop=(kc == 7),
                         reads=[wuk, hk], writes=[puk])
                sg, ug = sgb[fc % 2], ugb[fc % 2]
                k.act(sg[:, :W], pg[:, :W], AF.Silu, reads=[pgk], writes=[("sg", fc % 2)])
                if e < 16:
                    k.tt("dve", ug[:, :W], pu[:, :W], bcs[:, :W], ALU.mult, reads=[puk, "bcs"], writes=[("ug", fc % 2)])
                else:
                    k.copy("dve", ug[:, :W], pu[:, :W], reads=[puk], writes=[("ug", fc % 2)])
                k.tt("pool", act_t[:, fc, :W], sg[:, :W], ug[:, :W], ALU.mult,
                     reads=[("sg", fc % 2), ("ug", fc % 2)], writes=[ak])
            if pre and nsteps != 3:
                for p in ((0, 1, 2) if sidx == 0 else (3, 4, 5)):
                    piece_cast(n_ + 1, p)

        def down_part(st):
            n_, e, c, sidx, kk, off = st["n"], st["e"], st["c"], st["sidx"], st["k"], st["off"]
            t0, W = CH[c]
            b = n_ % 2
            wd = wdb[b]
            act_t = actb[kk % 2]
            ak = ("act", kk % 2)
            for dc in range(8):
                py, pyk = ps[4 + dc % 2], PSK[4 + dc % 2]
                wdk = ("wexp", b, 4 if dc < 4 else 5)
                for fc in range(4):
                    k.mm(py[:, :W], wd[:, fc, dc * 128:(dc + 1) * 128], act_t[:, fc, :W], start=(fc == 0), stop=(fc == 3),
                         reads=[wdk, ak], writes=[pyk])
                av = acc[:, dc, off:off + W]
                if e == 0:
                    k.copy("dve", av, py[:, :W], reads=[pyk], writes=[("acc", sidx, dc)])
                else:
                    k.tt("dve", av, py[:, :W], av, ALU.add, reads=[pyk, ("acc", sidx, dc)], writes=[("acc", sidx, dc)])

        def epilogue(si):
            o = 0
            for sidx, c in enumerate(scs[si]):
                t0, W = CH[c]
                col = 1 if c == 8 else 0
                off = o
                o += W
                k.dma(x1c[:, :, :W], sview(X1[l], c), reads=[("X1", c)], writes=["x1c"])
                for dc in range(8):
                    k.stt(x1c[:, dc, :W], acc[:, dc, off:off + W], modv3[:, 40 + dc, col:col + 1], x1c[:, dc, :W],
                          ALU.mult, ALU.add, reads=[("acc", sidx, dc), "x1c", "modc"], writes=["x1c"])
                if not last:
                    k.dma(sview(X2[l], c), x1c[:, :, :W], reads=["x1c"], writes=[("X2", c)])
                else:
                    k.act(sq[:, :, :W], x1c[:, :, :W], AF.Square, reads=["x1c"], writes=["fsq"])
                    for kc in range(8):
                        k.mm(ps[7][:, :W], ones_bf, sq[:, kc, :W], start=(kc == 0), stop=(kc == 7), reads=["fsq", "ones_bf"], writes=[PSK[7]])
                    k.act(lnv[:, :W], ps[7][:, :W], AF.Ln, bias=epsb[:, 0:1], scale=1.0 / D, reads=[PSK[7], "epsb"], writes=["flnv"])
                    k.act(rstd[:, :W], lnv[:, :W], AF.Exp, scale=-0.5, reads=["flnv"], writes=["frstd"])
                    for kc in range(8):
                        k.stt(acc[:, kc, off:off + W], x1c[:, kc, :W], gf_t[:, kc:kc + 1], rstd[:, :W], ALU.mult, ALU.mult,
                              reads=["x1c", "frstd", "gf_t", ("acc", sidx, kc)], writes=[("acc", sidx, kc)])
                    k.dma(outT[:, t0:t0 + W].rearrange("(k p) t -> p k t", p=128), acc[:, :, off:off + W],
                          reads=[("acc", sidx, kc) for kc in range(8)], writes=[("out", c)])

        for p in range(6):
            piece_dma(0, p)
            piece_cast(0, p)
        h2_dma(steps[0])
        gu_part(steps[0])
        for kk in range(len(steps)):
            st = steps[kk]
            if kk + 1 < len(steps):
                gu_part(steps[kk + 1])
            down_part(st)
            if st["e"] == 16 and st["sidx"] == st["nsteps"] - 1:
                epilogue(st["si"])

    P.emit()
    return nc


def _rope_tables():
    theta = 10000.0
    t = np.arange(SEQ)
    rows = (t // 64).astype(np.float64)
    cols = (t % 64).astype(np.float64)
    tabs = np.zeros((4, 128, NT), np.float32)
    tabs[0, :, :] = 1.0
    tabs[2, :, :] = 1.0

    def fill(ci, si, r0, nf):
        inv = theta ** (-np.arange(nf, dtype=np.float64) / nf)
        for half, pos in enumerate((rows, cols)):
            ang = pos[None, :] * inv[:, None]
            b = r0 + half * 2 * nf
            tabs[ci, b:b + nf, :SEQ] = np.cos(ang)
            tabs[ci, b + nf:b + 2 * nf, :SEQ] = np.cos(ang)
            tabs[si, b:b + nf, :SEQ] = -np.sin(ang)
            tabs[si, b + nf:b + 2 * nf, :SEQ] = np.sin(ang)

    fill(0, 1, 0, 16)
    fill(2, 3, 64, 8)
    return tabs


def _swap_perm(n, nf):
    p = np.arange(n)
    out = p.copy()
    for b in range(0, n, 2 * nf):
        out[b:b + nf] = p[b + nf:b + 2 * nf]
        out[b + nf:b + 2 * nf] = p[b:b + nf]
    return out


def _masks():
    a = np.arange(128)
    lo = (a[None, :] <= a[:, None]).astype(np.float32)
    hi = (a[None, :] >= a[:, None]).astype(np.float32)
    one = np.ones((128, 128), np.float32)
    zero = np.zeros((128, 128), np.float32)
    m = np.zeros((128, 6, 512), np.float32)
    for r in range(6):
        rel = r - 1
        for i in range(4):
            d = rel - i
            blk = one if d == 0 else lo if d == -1 else hi if d == 1 else zero
            m[:, r, i * 128:(i + 1) * 128] = blk
    return m.reshape(128, 6 * 512)


_NC_CACHE = {}


def kernel(x, c, ctx, c_ctx, w_mod, b_mod, norm_mix_g, norm_ffn_g, w_in, w_out, swa_sink,
           glb_q_gain, glb_k_gain, mla_q_gain, mla_w_uq, mla_kv_gain, mla_w_ukv,
           router_w, router_bias, exp_w_gate, exp_w_up, exp_w_down,
           shr_w_gate, shr_w_up, shr_w_down, final_norm_g, _dbg=False, _cores=None):
    f = lambda a: np.ascontiguousarray(np.asarray(a, dtype=np.float32))
    x, c, ctx, c_ctx = f(x), f(c), f(ctx), f(c_ctx)
    w_in = f(w_in)
    perm = np.arange(1696)
    p64 = _swap_perm(64, 16)
    for hb_ in list(range(0, 640, 64)) + list(range(640, 1280, 64)):
        perm[hb_:hb_ + 64] = hb_ + p64
    perm[1664:1696] = 1664 + _swap_perm(32, 8)
    w_in_sw = np.ascontiguousarray(w_in[:, :, perm])
    w_uq = f(mla_w_uq)
    pu = np.arange(384)
    for h in range(4):
        pu[h * 96 + 64:h * 96 + 96] = h * 96 + 64 + _swap_perm(32, 8)
    w_uq_sw = np.ascontiguousarray(w_uq[:, :, pu])
    gq, gk = f(glb_q_gain), f(glb_k_gain)
    gqk = np.stack([gq, gq[:, p64], gk, gk[:, p64]], axis=-1)
    gmq = np.ascontiguousarray(f(mla_q_gain).reshape(NL, 2, 128).transpose(0, 2, 1))
    gmkv = f(mla_kv_gain).reshape(NL, 128, 1)
    bm = np.ascontiguousarray(f(b_mod).reshape(NL, 48, 128).transpose(0, 2, 1))
    g1 = np.ascontiguousarray(f(norm_mix_g).reshape(NL, 8, 128).transpose(0, 2, 1))
    g2 = np.ascontiguousarray(f(norm_ffn_g).reshape(NL, 8, 128).transpose(0, 2, 1))
    gfin = np.ascontiguousarray(f(final_norm_g).reshape(8, 128).T)
    rw = np.ascontiguousarray(f(router_w).reshape(8, 128, 16).transpose(1, 0, 2).reshape(128, 128))
    shared = {
        "w_mod": f(w_mod), "bm": bm, "g1": g1, "g2": g2, "gf": gfin, "w_in": w_in, "w_in_sw": w_in_sw,
        "w_out": f(w_out), "sink": f(swa_sink).reshape(NL, 1, 6), "gqk": np.ascontiguousarray(gqk), "gmq": gmq, "gmkv": gmkv,
        "w_uq": w_uq, "w_uq_sw": w_uq_sw, "w_ukv": f(mla_w_ukv), "rw": rw, "rb": f(router_bias),
        "ewg": f(exp_w_gate), "ewu": f(exp_w_up), "ewd": f(exp_w_down),
        "swg": f(shr_w_gate), "swu": f(shr_w_up), "swd": f(shr_w_down),
        "tabs": _rope_tables(), "masks": _masks(), "ident": np.eye(128, dtype=np.float32),
    }
    cores = list(range(8)) if _cores is None else _cores
    in_maps = []
    for b in cores:
        m = dict(shared)
        m["xT"] = np.ascontiguousarray(x[b].T)
        m["ctxT"] = np.ascontiguousarray(ctx[b].T)
        ccv = np.stack([c[b], c_ctx], axis=-1).reshape(8, 128, 2).transpose(1, 0, 2).reshape(128, 16)
        m["cc"] = np.ascontiguousarray(ccv)
        in_maps.append(m)
    key = bool(_dbg)
    if key not in _NC_CACHE:
        _NC_CACHE[key] = build_program(dbg=_dbg)
    nc = _NC_CACHE[key]
    res = run_bass_kernel_spmd(nc, in_maps, core_ids=list(range(len(cores))))
    if _dbg:
        return res
    out = np.stack([np.ascontiguousarray(r["outT"].T) for r in res.results], axis=0)
    return out.astype(np.float32)
```
